# Optimizing a Trainium2 kernel written in Bass

```python
import jax, jax.numpy as jnp
from jax import lax
import numpy as np

D_MODEL = 4096
BATCH = 1
SEQ = 8192
DEPTH = 1

GRID_W = 64
CTX_LEN = 256
EPS = 1e-6

D_MIX = D_MODEL
D_GLA = D_MIX // 2
D_CONV = D_MIX - D_GLA
GLA_HEADS = 16
GLA_DK = D_GLA // 2
GLA_HK = GLA_DK // GLA_HEADS
GLA_HV = D_GLA // GLA_HEADS
GATE_RANK = 16
GATE_TAU = 16.0
GLA_CHUNK = 64
CONV_WIDTH = 31
CONV_PAD = (CONV_WIDTH - 1) // 2

OFF_Q = 0
OFF_K = OFF_Q + GLA_DK
OFF_V = OFF_K + GLA_DK
OFF_R = OFF_V + D_GLA
OFF_GF = OFF_R + D_GLA
OFF_GB = OFF_GF + GATE_RANK
OFF_CA = OFF_GB + GATE_RANK
OFF_CB = OFF_CA + D_CONV
IN_COLS = OFF_CB + D_CONV

N_EXPERTS = 64
D_EXPERT = 512
D_SHARED = 512
TOP_K = 8
N_GROUPS = 8
TOPK_GROUPS = 4
ROUTED_SCALE = 2.5
MOE_BLOCK = 128

kernel_name = "hybrid_gla_conformer_moe_dit"


def rmsnorm(x, g):
    xf = x.astype(jnp.float32)
    y = xf * lax.rsqrt(jnp.mean(xf * xf, axis=-1, keepdims=True) + EPS) * g.astype(jnp.float32)
    return y.astype(x.dtype)


def modulate(h, shift, scale):
    return h * (1 + scale) + shift


def gla_scan(q, k, v, logf, s0, with_output):
    B, H, T, dk = q.shape
    L = GLA_CHUNK
    nc = T // L
    f32 = jnp.float32

    def to_chunks(a):
        return jnp.moveaxis(a.astype(f32).reshape(B, H, nc, L, a.shape[-1]), 2, 0)

    mask = jnp.tril(jnp.ones((L, L), dtype=bool))[:, :, None]

    def step(S, inp):
        qc, kc, vc, gc = inp
        b = jnp.cumsum(gc, axis=2)
        bL = b[:, :, -1, :]
        S_new = jnp.exp(bL)[..., None] * S + jnp.einsum(
            'bhsd,bhse->bhde', kc * jnp.exp(bL[:, :, None, :] - b), vc)
        if not with_output:
            return S_new, None
        o_inter = jnp.einsum('bhtd,bhde->bhte', qc * jnp.exp(b), S)
        diff = b[:, :, :, None, :] - b[:, :, None, :, :]
        decay = jnp.exp(jnp.where(mask, diff, -jnp.inf))
        att = jnp.einsum('bhtd,bhsd,bhtsd->bhts', qc, kc, decay)
        o = o_inter + jnp.einsum('bhts,bhse->bhte', att, vc)
        return S_new, o

    S, o = lax.scan(step, s0.astype(f32), (to_chunks(q), to_chunks(k), to_chunks(v), to_chunks(logf)))
    if not with_output:
        return None, S
    o = jnp.moveaxis(o, 0, 2).reshape(B, H, T, v.shape[-1])
    return o, S


def gla_branch(proj, w_gate_up, b_gate_up, g_out, s_fwd, s_bwd, with_output):
    B, T, _ = proj.shape

    def heads(a, dh):
        return a.reshape(B, T, GLA_HEADS, dh).transpose(0, 2, 1, 3)

    q = heads(proj[..., OFF_Q:OFF_K], GLA_HK) * GLA_HK ** -0.5
    k = heads(proj[..., OFF_K:OFF_V], GLA_HK)
    v = heads(proj[..., OFF_V:OFF_R], GLA_HV)

    def log_decay(low, d):
        z = (low @ w_gate_up[d] + b_gate_up[d]).astype(jnp.float32)
        return heads(jax.nn.log_sigmoid(z) / GATE_TAU, GLA_HK)

    lf = log_decay(proj[..., OFF_GF:OFF_GB], 0)
    lb = log_decay(proj[..., OFF_GB:OFF_CA], 1)
    flip = lambda a: jnp.flip(a, axis=2)
    o_f, s_f = gla_scan(q, k, v, lf, s_fwd, with_output)
    o_b, s_b = gla_scan(flip(q), flip(k), flip(v), flip(lb), s_bwd, with_output)
    if not with_output:
        return None, (s_f, s_b)
    o = o_f + flip(o_b)
    o = o * lax.rsqrt(jnp.mean(o * o, axis=-1, keepdims=True) + EPS)
    o = o.transpose(0, 2, 1, 3).reshape(B, T, D_GLA) * g_out.astype(jnp.float32)
    r = proj[..., OFF_R:OFF_GF].astype(jnp.float32)
    return (o * jax.nn.silu(r)).astype(proj.dtype), (s_f, s_b)


def conv_branch(proj, w_dw, b_dw, g_ln, b_ln, rows):
    u = proj[..., OFF_CA:OFF_CB] * jax.nn.sigmoid(proj[..., OFF_CB:IN_COLS])
    B, T, C = u.shape
    seqs = u.reshape(B * rows, GRID_W, C) if rows is not None else u
    y = lax.conv_general_dilated(seqs, w_dw[:, None, :].astype(seqs.dtype), window_strides=(1,),
                                 padding=[(CONV_PAD, CONV_PAD)],
                                 dimension_numbers=('NWC', 'WIO', 'NWC'), feature_group_count=C)
    y = y.reshape(B, T, C).astype(jnp.float32) + b_dw.astype(jnp.float32)
    mu = jnp.mean(y, axis=-1, keepdims=True)
    var = jnp.mean(jnp.square(y - mu), axis=-1, keepdims=True)
    y = (y - mu) * lax.rsqrt(var + EPS) * g_ln.astype(jnp.float32) + b_ln.astype(jnp.float32)
    return jax.nn.silu(y).astype(proj.dtype)


def moe(h, w_router, b_router, w_e_gate, w_e_up, w_e_down, w_s_gate, w_s_up, w_s_down):
    B, T, D = h.shape
    N = B * T
    xt = h.reshape(N, D)
    scores = jax.nn.sigmoid((xt @ w_router).astype(jnp.float32))
    sel = scores + b_router.astype(jnp.float32)
    grp_score = lax.top_k(sel.reshape(N, N_GROUPS, N_EXPERTS // N_GROUPS), 2)[0].sum(-1)
    _, gidx = lax.top_k(grp_score, TOPK_GROUPS)
    gmask = jax.nn.one_hot(gidx, N_GROUPS, dtype=jnp.float32).sum(-2) > 0
    emask = jnp.repeat(gmask, N_EXPERTS // N_GROUPS, axis=1)
    _, eidx = lax.top_k(jnp.where(emask, sel, -jnp.inf), TOP_K)
    w = jnp.take_along_axis(scores, eidx, axis=1)
    w = w / jnp.sum(w, axis=-1, keepdims=True) * ROUTED_SCALE

    NK = N * TOP_K
    NB = (NK + MOE_BLOCK - 1) // MOE_BLOCK + N_EXPERTS
    flat_e = eidx.reshape(-1).astype(jnp.int32)
    flat_tok = jnp.repeat(jnp.arange(N, dtype=jnp.int32), TOP_K)
    flat_w = w.reshape(-1)
    order = jnp.argsort(flat_e)
    se = flat_e[order]
    counts = jnp.bincount(flat_e, length=N_EXPERTS).astype(jnp.int32)
    start = jnp.cumsum(counts) - counts
    pcounts = (counts + MOE_BLOCK - 1) // MOE_BLOCK * MOE_BLOCK
    pend = jnp.cumsum(pcounts)
    pstart = pend - pcounts
    dest = pstart[se] + (jnp.arange(NK, dtype=jnp.int32) - start[se])
    slot_tok = jnp.full((NB * MOE_BLOCK,), N, dtype=jnp.int32).at[dest].set(flat_tok[order])
    slot_w = jnp.zeros((NB * MOE_BLOCK,), jnp.float32).at[dest].set(flat_w[order])
    blk_e = jnp.minimum(jnp.searchsorted(pend, jnp.arange(NB, dtype=jnp.int32) * MOE_BLOCK, side='right'),
                        N_EXPERTS - 1)
    xpad = jnp.concatenate([xt, jnp.zeros((1, D), xt.dtype)], axis=0)

    def body(y, inp):
        e, tok, wt = inp
        xb = xpad[tok]
        hb = jax.nn.silu(xb @ w_e_gate[e]) * (xb @ w_e_up[e])
        ob = (hb @ w_e_down[e]).astype(jnp.float32) * wt[:, None]
        return y.at[tok].add(ob), None

    y, _ = lax.scan(body, jnp.zeros((N + 1, D), jnp.float32),
                    (blk_e, slot_tok.reshape(NB, MOE_BLOCK), slot_w.reshape(NB, MOE_BLOCK)))
    shared = (jax.nn.silu(xt @ w_s_gate) * (xt @ w_s_up)) @ w_s_down
    return (y[:N] + shared.astype(jnp.float32)).astype(h.dtype).reshape(B, T, D)


def setup_inputs(seed: int = 0) -> dict:
    key = jax.random.key(seed)
    ks = jax.random.split(key, 28)
    f32 = jnp.float32
    nrm = lambda k, shape, s: jax.random.normal(k, shape, f32) * s
    L, D = DEPTH, D_MODEL
    return {
        "x": nrm(ks[0], (BATCH, SEQ, D), 1.0),
        "c": nrm(ks[1], (BATCH, D), 1.0),
        "ctx": nrm(ks[2], (BATCH, CTX_LEN, D), 1.0),
        "c_ctx": nrm(ks[3], (D,), 1.0),
        "w_ada": nrm(ks[4], (L, D, 6 * D), 0.5 * D ** -0.5),
        "b_ada": nrm(ks[5], (L, 6 * D), 0.02),
        "g_norm_mix": 1.0 + nrm(ks[6], (L, D), 0.02),
        "g_norm_ffn": 1.0 + nrm(ks[7], (L, D), 0.02),
        "w_in": nrm(ks[8], (L, D, IN_COLS), D ** -0.5),
        "w_gate_up": nrm(ks[9], (L, 2, GATE_RANK, GLA_DK), GATE_RANK ** -0.5),
        "b_gate_up": nrm(ks[10], (L, 2, GLA_DK), 0.1),
        "g_gla_out": 1.0 + nrm(ks[11], (L, D_GLA), 0.02),
        "w_dw": nrm(ks[12], (L, CONV_WIDTH, D_CONV), CONV_WIDTH ** -0.5),
        "b_dw": nrm(ks[13], (L, D_CONV), 0.02),
        "g_conv_ln": 1.0 + nrm(ks[14], (L, D_CONV), 0.02),
        "b_conv_ln": nrm(ks[15], (L, D_CONV), 0.02),
        "w_out": nrm(ks[16], (L, D_MIX, D), D_MIX ** -0.5),
        "w_router": nrm(ks[17], (L, D, N_EXPERTS), D ** -0.5),
        "b_router": nrm(ks[18], (L, N_EXPERTS), 0.01),
        "w_e_gate": nrm(ks[19], (L, N_EXPERTS, D, D_EXPERT), D ** -0.5),
        "w_e_up": nrm(ks[20], (L, N_EXPERTS, D, D_EXPERT), D ** -0.5),
        "w_e_down": nrm(ks[21], (L, N_EXPERTS, D_EXPERT, D), D_EXPERT ** -0.5),
        "w_s_gate": nrm(ks[22], (L, D, D_SHARED), D ** -0.5),
        "w_s_up": nrm(ks[23], (L, D, D_SHARED), D ** -0.5),
        "w_s_down": nrm(ks[24], (L, D_SHARED, D), D_SHARED ** -0.5),
        "g_final": 1.0 + nrm(ks[25], (D,), 0.02),
    }


def reference(x, c, ctx, c_ctx, w_ada, b_ada, g_norm_mix, g_norm_ffn, w_in, w_gate_up, b_gate_up,
              g_gla_out, w_dw, b_dw, g_conv_ln, b_conv_ln, w_out, w_router, b_router,
              w_e_gate, w_e_up, w_e_down, w_s_gate, w_s_up, w_s_down, g_final):
    B = x.shape[0]
    rows = x.shape[1] // GRID_W
    x_lat, x_ctx = x, ctx
    zero_state = jnp.zeros((B, GLA_HEADS, GLA_HK, GLA_HV), jnp.float32)
    for l in range(DEPTH):
        last = l == DEPTH - 1
        mod_l = (jax.nn.silu(c) @ w_ada[l] + b_ada[l])[:, None, :]
        mod_c = jax.nn.silu(c_ctx) @ w_ada[l] + b_ada[l]
        sh_m, sc_m, ga_m, sh_f, sc_f, ga_f = jnp.split(mod_l, 6, axis=-1)
        csh_m, csc_m, cga_m, csh_f, csc_f, cga_f = jnp.split(mod_c, 6, axis=-1)

        h_c = modulate(rmsnorm(x_ctx, g_norm_mix[l]), csh_m, csc_m)
        proj_c = h_c @ w_in[l]
        o_gla_c, (s_f, s_b) = gla_branch(proj_c, w_gate_up[l], b_gate_up[l], g_gla_out[l],
                                         zero_state, zero_state, not last)

        h_l = modulate(rmsnorm(x_lat, g_norm_mix[l]), sh_m, sc_m)
        proj_l = h_l @ w_in[l]
        o_gla_l, _ = gla_branch(proj_l, w_gate_up[l], b_gate_up[l], g_gla_out[l], s_f, s_b, True)
        o_conv_l = conv_branch(proj_l, w_dw[l], b_dw[l], g_conv_ln[l], b_conv_ln[l], rows)
        mix_l = jnp.concatenate([o_gla_l, o_conv_l], axis=-1) @ w_out[l]
        x_lat = x_lat + ga_m * mix_l

        h_l = modulate(rmsnorm(x_lat, g_norm_ffn[l]), sh_f, sc_f)
        x_lat = x_lat + ga_f * moe(h_l, w_router[l], b_router[l], w_e_gate[l], w_e_up[l], w_e_down[l],
                                   w_s_gate[l], w_s_up[l], w_s_down[l])

        if not last:
            o_conv_c = conv_branch(proj_c, w_dw[l], b_dw[l], g_conv_ln[l], b_conv_ln[l], None)
            x_ctx = x_ctx + cga_m * (jnp.concatenate([o_gla_c, o_conv_c], axis=-1) @ w_out[l])
            h_c = modulate(rmsnorm(x_ctx, g_norm_ffn[l]), csh_f, csc_f)
            x_ctx = x_ctx + cga_f * moe(h_c, w_router[l], b_router[l], w_e_gate[l], w_e_up[l], w_e_down[l],
                                       w_s_gate[l], w_s_up[l], w_s_down[l])
    return rmsnorm(x_lat, g_final)
```

```python
import contextlib
import numpy as np
import concourse.bass as bass
import concourse.mybir as mybir
from concourse.bass_utils import run_bass_kernel_spmd

F32 = mybir.dt.float32
BF16 = mybir.dt.bfloat16
I32 = mybir.dt.int32
ALU = mybir.AluOpType
AF = mybir.ActivationFunctionType
AX = mybir.AxisListType

EPS = 1e-6
GATE_TAU = 16.0
CONV_W = 31
TOPK = 8
NGRP = 8
TOPG = 4
RSCALE = 2.5


class _Op:
    __slots__ = ("eng", "fn", "reads", "writes", "dma", "deps", "needed", "cnt", "sem", "target")

    def __init__(self, eng, fn, reads, writes, dma):
        self.eng = eng; self.fn = fn; self.reads = reads; self.writes = writes; self.dma = dma
        self.deps = (); self.needed = False; self.cnt = 0; self.sem = None; self.target = 0


class Sched:
    QUEUES = ("pe", "act", "dve", "pool", "sp")

    def __init__(self, nc, stack, ndma_sems=12):
        self.nc = nc
        self.E = {"pe": nc.tensor, "act": nc.scalar, "dve": nc.vector, "pool": nc.gpsimd, "sp": nc.sync}
        self.ops = []
        self.esem = {q: stack.enter_context(nc.semaphore("e_" + q)) for q in ("pe", "act", "dve", "pool")}
        self.dsem = {q: [stack.enter_context(nc.semaphore("d_%s%d" % (q, i))) for i in range(ndma_sems)]
                     for q in ("sp", "pool")}
        self.cnt = {q: 0 for q in self.esem}
        self.ndma = {q: 0 for q in self.dsem}
        self.hist = {q: [] for q in self.dsem}
        self.stats = dict(ops=0, waits=0)

    def op(self, eng, fn, reads=(), writes=()):
        self.ops.append(_Op(eng, fn, tuple(reads), tuple(writes), False))

    def dma(self, eng, fn, reads=(), writes=()):
        self.ops.append(_Op(eng, fn, tuple(reads), tuple(writes), True))

    def flush(self, barrier=True):
        ops = self.ops
        self.ops = []
        last_w = {}; readers = {}
        for i, o in enumerate(ops):
            d = set()
            for k in o.reads:
                if k in last_w: d.add(last_w[k])
            for k in o.writes:
                if k in last_w: d.add(last_w[k])
                for r in readers.get(k, ()): d.add(r)
            d.discard(i)
            o.deps = d
            for k in o.writes:
                last_w[k] = i; readers[k] = []
            for k in o.reads:
                readers.setdefault(k, []).append(i)
        seen = {q: {} for q in self.QUEUES}
        seen_dma = {q: set() for q in self.QUEUES}
        lastc = {}
        for i, o in enumerate(ops):
            keep = []; byeng = {}
            for j in o.deps:
                p = ops[j]
                if p.dma:
                    if j not in seen_dma[o.eng]: keep.append(j)
                else:
                    if p.eng == "pe" and o.eng == "pe": continue
                    byeng[p.eng] = max(byeng.get(p.eng, -1), j)
            for e, j in byeng.items():
                if seen[o.eng].get(e, -1) >= j: continue
                seen[o.eng][e] = j
                keep.append(j)
            for j in keep:
                ops[j].needed = True
                if ops[j].dma: seen_dma[o.eng].add(j)
            o.deps = sorted(keep)
            if not o.dma: lastc[o.eng] = i
        if barrier:
            for j in lastc.values(): ops[j].needed = True
        for i, o in enumerate(ops):
            eng = self.E[o.eng]
            for j in o.deps:
                p = ops[j]
                if p.dma: eng.wait_ge(p.sem, p.target)
                else: eng.wait_ge(self.esem[p.eng], p.cnt)
                self.stats["waits"] += 1
            if o.dma:
                n = self.ndma[o.eng]; K = len(self.dsem[o.eng])
                o.sem = self.dsem[o.eng][n % K]; o.target = 16 * (n // K + 1)
                if n >= K: eng.wait_ge(o.sem, 16 * (n // K))
                self.ndma[o.eng] = n + 1
                o.fn().then_inc(o.sem, 16)
                self.hist[o.eng].append((o.sem, o.target))
                if len(self.hist[o.eng]) > K: self.hist[o.eng].pop(0)
            else:
                ins = o.fn()
                if o.needed:
                    self.cnt[o.eng] += 1; o.cnt = self.cnt[o.eng]
                    ins.then_inc(self.esem[o.eng], 1)
            self.stats["ops"] += 1
        if barrier:
            for q in self.QUEUES:
                eng = self.E[q]
                for e in self.esem:
                    if e != q and self.cnt[e] > 0: eng.wait_ge(self.esem[e], self.cnt[e])
                for dq in self.dsem:
                    for (sem, tgt) in self.hist[dq]: eng.wait_ge(sem, tgt)


class Cfg:
    def __init__(self, D=4096, SEQ=8192, CTX=256, NCORES=8, E=64, DE=512, DS=512, C=512, GRID_W=64):
        self.D = D; self.SEQ = SEQ; self.CTX = CTX; self.NCORES = NCORES
        self.E = E; self.DE = DE; self.DS = DS; self.C = C; self.GRID_W = GRID_W
        self.TOK = SEQ // NCORES; self.NT = self.TOK // 128; self.KD = D // 128
        self.DG = D // 2; self.NH = self.DG // 128; self.NP = self.NH // 2; self.DK = self.NH * 64
        self.DC = D - self.DG; self.NCC = self.DC // 128
        self.NTB = min(512, self.TOK)
        self.NS = NCORES - 1
        self.CT = CTX // 128
        self.NB = C // 128
        self.NHB = DE // 128; self.NSB = DS // 128
        self.DQ = min(2048, D); self.NQ = D // self.DQ; self.DW = min(512, self.DQ)
        self.EPG = E // NGRP


def _cm_layout(c):
    names = [("ident", 128), ("McF", 128), ("McB", 128), ("M1F", 128), ("M1B", 128), ("maskF2", 256),
             ("maskB2", 256), ("ones", 128), ("ustr", 128), ("iotaC", c.C), ("tokid1", c.NT),
             ("trash", 1), ("flags", 3 * (c.NS + 1)), ("negcol", 1)]
    off = {}; o = 0
    for n, w in names:
        off[n] = (o, w); o += w
    return off, o


def _make_consts(c, core):
    off, tot = _cm_layout(c)
    cm = np.zeros((128, tot), np.float32)
    s = np.arange(128)[:, None]; t = np.arange(128)[None, :]
    g = -1.0 / GATE_TAU
    def put(n, a): cm[:, off[n][0]:off[n][0] + off[n][1]] = a
    put("ident", (s == t))
    put("McF", (s <= t) * g); put("McB", (s >= t) * g)
    put("M1F", (s > t) * g); put("M1B", (s < t) * g)
    mf = (s <= t).astype(np.float32); mb = (s >= t).astype(np.float32)
    put("maskF2", np.concatenate([mf, mf], 1)); put("maskB2", np.concatenate([mb, mb], 1))
    put("ones", np.ones((128, 128))); put("ustr", (s < t))
    put("iotaC", np.broadcast_to(np.arange(c.C)[None, :], (128, c.C)))
    put("tokid1", np.arange(c.NT)[None, :] * 128 + np.arange(128)[:, None] + 1)
    put("trash", c.TOK + np.arange(128)[:, None])
    fl = np.zeros((3 * (c.NS + 1),), np.float32)
    for b in range(c.NS + 1):
        fl[3 * b + 0] = 1.0 if b == core else 0.0
        fl[3 * b + 1] = 0.0 if b == core else 1.0
        fl[3 * b + 2] = 1.0 if b == core else 0.0
    put("flags", np.broadcast_to(fl[None, :], (128, fl.size)))
    put("negcol", np.full((128, 1), g))
    return cm


def build(c, stop=None, dbg=False):
    nc = bass.Bass("TRN2", target_bir_lowering=False)
    off, CMW = _cm_layout(c)
    D, KD, TOK, NT, NTB = c.D, c.KD, c.TOK, c.NT, c.NTB
    DK, DG, DC, NP, NCC, E, C, NB = c.DK, c.DG, c.DC, c.NP, c.NCC, c.E, c.C, c.NB
    TPB = NTB // 128
    KP = min(8, KD)

    def din(name, shape, dt=F32):
        return nc.dram_tensor(name, list(shape), dt, kind="ExternalInput").ap()

    def dscr(name, shape, dt=F32):
        if dbg:
            return nc.dram_tensor(name, list(shape), dt, kind="ExternalOutput").ap()
        return nc.dram_tensor(name, list(shape), dt).ap()

    x_own = din("x_own", [TOK, D]); x_oth = din("x_oth", [max(c.NS, 1) * TOK, D]); ctx2 = din("ctx2", [2 * c.CTX, D])
    cc = din("cc", [2, D]); w_ada = din("w_ada", [D, 6 * D]); b_ada = din("b_ada", [6 * D])
    gvecs = din("gvecs", [3, D])
    w_q = din("w_q", [D, DK]); w_k = din("w_k", [D, DK]); w_v = din("w_v", [D, DG]); w_r = din("w_r", [D, DG])
    w_ab = din("w_ab", [D, 2 * DC]); w_g = din("w_g", [D, 32])
    w_gs = din("w_gs", [(c.NS + 2) * D, 16]); wgu_s = din("wgu_s", [(c.NS + 2) * 16, DK]); bgu_s = din("bgu_s", [c.NS + 2, DK])
    wgu = din("wgu", [32, DK]); bgu = din("bgu", [2, DK])
    g_gla = din("g_gla", [DG]); w_dw = din("w_dw", [CONV_W, DC]); cvec = din("cvec", [3, DC])
    w_out = din("w_out", [D, D]); w_rt = din("w_rt", [D, E]); b_rt = din("b_rt", [E])
    w_eg = din("w_eg", [E * c.NHB * 128, KD * 128]); w_eu = din("w_eu", [E * c.NHB * 128, KD * 128])
    w_ed = din("w_ed", [E * c.DE, D])
    w_sg = din("w_sg", [D, c.DS]); w_su = din("w_su", [D, c.DS]); w_sd = din("w_sd", [c.DS, D])
    cm_in = din("cm", [128, CMW])
    out = nc.dram_tensor("out", [TOK, D], F32, kind="ExternalOutput").ap()

    mod_d = dscr("mod_d", [2, 6 * D])
    qT_d = dscr("qT_d", [NP * 128, TOK], BF16); kT_d = dscr("kT_d", [NP * 128, TOK], BF16)
    k_d = dscr("k_d", [TOK, DK], BF16); v_d = dscr("v_d", [TOK, DG], BF16); sr_d = dscr("sr_d", [TOK, DG], BF16)
    u_d = dscr("u_d", [NCC * 128, TOK], BF16); ob_d = dscr("ob_d", [TOK, DG])
    xlat_d = dscr("xlat_d", [TOK, D]); xs2_d = dscr("xs2_d", [TOK, D], BF16)
    y_q = [dscr("y_q%d" % q, [TOK + 128, c.DQ]) for q in range(c.NQ)]
    NBLKA = (DK + 255) // 256 + (DG + 255) // 256
    wkv_c = nc.dram_tensor("wkv_c", [NBLKA * (KD // KP) * 128, KP * 256], BF16).ap()

    ucnt = [0]

    def uname(n):
        ucnt[0] += 1
        return "s%d_%s" % (ucnt[0], n)

    top = contextlib.ExitStack()
    with top:
        S = Sched(nc, top)
        cm = top.enter_context(nc.sbuf_tensor("s_cm", [128, CMW], F32))
        idb = top.enter_context(nc.sbuf_tensor("s_idb", [128, 128], BF16))
        colA = top.enter_context(nc.sbuf_tensor("s_colA", [128, 4 * KD], F32))
        colB = top.enter_context(nc.sbuf_tensor("s_colB", [128, 4 * KD], F32))
        Gs = top.enter_context(nc.sbuf_tensor("s_Gs", [128, 3, KD], F32))
        gla_scope = top.enter_context(contextlib.ExitStack())
        S0 = gla_scope.enter_context(nc.sbuf_tensor("s_S0", [128, 2, NP * 256], F32))
        lowT = gla_scope.enter_context(nc.sbuf_tensor("s_lowT", [16, 2, TOK], BF16))

        def CM(n, lo=0, hi=None):
            o, w = off[n]
            hi = w if hi is None else hi
            return cm[:, o + lo:o + hi]
        ident = CM("ident")
        Gm = Gs[:, 0, :]; cGm = Gs[:, 1, :]; Gf = Gs[:, 2, :]
        shm = colA[:, 2 * KD:3 * KD]; cshm = colB[:, 3 * KD:4 * KD]; shf = colB[:, 2 * KD:3 * KD]

        V = lambda fn, r=(), w=(): S.op("dve", fn, r, w)
        A = lambda fn, r=(), w=(): S.op("act", fn, r, w)
        G = lambda fn, r=(), w=(): S.op("pool", fn, r, w)
        P = lambda fn, r=(), w=(): S.op("pe", fn, r, w)
        DM = lambda fn, r=(), w=(): S.dma("sp", fn, r, w)
        GD = lambda fn, r=(), w=(): S.dma("pool", fn, r, w)
        alt = [0]

        def VA(fv, fa, r=(), w=()):
            alt[0] ^= 1
            if alt[0]: V(fv, r, w)
            else: A(fa, r, w)

        DM(lambda: nc.sync.dma_start(out=cm[:], in_=cm_in), w=["cm"])
        V(lambda: nc.vector.tensor_copy(out=idb[:], in_=ident), r=["cm"], w=["idb"])
        S.flush()

        with contextlib.ExitStack() as ph:
            T = lambda n, s, d=F32: ph.enter_context(nc.sbuf_tensor(uname(n), list(s), d))
            ps = ph.enter_context(nc.psum_tensor("ps0", [128, 8, 512], F32))
            cst = T("cst", [128, 128]); sct = T("sct", [128, 128]); scT2 = T("scT2", [128, KD, 2])
            wst = [T("wst%d" % i, [128, KP, 512]) for i in range(3)]
            brow = [T("brow%d" % i, [2, 512]) for i in range(2)]
            mst = [T("mst%d" % i, [2, 512]) for i in range(2)]
            stk = [T("stk%d" % i, [128, 128]) for i in range(2)]
            V(lambda: nc.vector.memset(cst[:], 0.0), w=["cst"])
            for v in range(2):
                DM(lambda v=v: nc.sync.dma_start(out=cst[v * KD:(v + 1) * KD, :], in_=cc[v].rearrange("(k p) -> k p", p=128)),
                   w=["cst"])
            A(lambda: nc.scalar.activation(out=cst[:], in_=cst[:], func=AF.Silu), r=["cst"], w=["cst"])
            P(lambda: nc.tensor.transpose(out=ps[:, 7, 0:128], in_=cst[:], identity=ident), r=["cst"], w=[("bk", 7)])
            V(lambda: nc.vector.tensor_copy(out=sct[:], in_=ps[:, 7, 0:128]), r=[("bk", 7)], w=["sct"])
            for v in range(2):
                V(lambda v=v: nc.vector.tensor_copy(out=scT2[:, :, v], in_=sct[:, v * KD:(v + 1) * KD]), r=["sct"], w=["scT2"])
            NBLK = 6 * D // 512
            wi = 0
            for j in range(NBLK):
                bank = j % 4
                DM(lambda j=j: nc.sync.dma_start(out=brow[j % 2][:], in_=b_ada[j * 512:(j + 1) * 512].partition_broadcast(2)),
                   w=[("brow", j % 2)])
                for kp in range(KD // KP):
                    r_ = wi % 3; wi += 1
                    DM(lambda j=j, kp=kp, r_=r_: nc.sync.dma_start(
                        out=wst[r_][:], in_=w_ada[kp * KP * 128:(kp + 1) * KP * 128, j * 512:(j + 1) * 512]
                        .rearrange("(k p) n -> p k n", p=128)), w=[("wst", r_)])
                    for kk in range(KP):
                        k = kp * KP + kk
                        P(lambda k=k, kk=kk, r_=r_, bank=bank: nc.tensor.matmul(
                            out=ps[0:2, bank, :], lhsT=scT2[:, k, :], rhs=wst[r_][:, kk, :],
                            start=(k == 0), stop=(k == KD - 1)), r=[("wst", r_), "scT2"], w=[("pb", bank)])
                V(lambda j=j, bank=bank: nc.vector.tensor_tensor(out=mst[j % 2][:], in0=ps[0:2, bank, :], in1=brow[j % 2][:], op=ALU.add),
                  r=[("pb", bank), ("brow", j % 2)], w=[("mst", j % 2)])
                DM(lambda j=j: nc.sync.dma_start(out=mod_d[:, j * 512:(j + 1) * 512], in_=mst[j % 2][:]),
                   r=[("mst", j % 2)], w=["mod_d"])
            rows = lambda ap: ap.rearrange("(k p) -> k p", p=128)
            vecsA = [gvecs[0], mod_d[0, D:2 * D], mod_d[0, 0:D], mod_d[1, D:2 * D]]
            vecsB = [gvecs[1], mod_d[0, 4 * D:5 * D], mod_d[0, 3 * D:4 * D], mod_d[1, 0:D]]
            for si, (vecs, col) in enumerate(((vecsA, colA), (vecsB, colB))):
                V(lambda si=si: nc.vector.memset(stk[si][:], 0.0), w=[("stk", si)])
                for vi, vap in enumerate(vecs):
                    DM(lambda si=si, vi=vi, vap=vap: nc.sync.dma_start(out=stk[si][vi * KD:(vi + 1) * KD, :], in_=rows(vap)),
                       r=["mod_d"], w=[("stk", si)])
                P(lambda si=si: nc.tensor.transpose(out=ps[:, 6, si * 128:(si + 1) * 128], in_=stk[si][:], identity=ident),
                  r=[("stk", si)], w=[("bk", 6)])
                V(lambda si=si, col=col: nc.vector.tensor_copy(out=col[:], in_=ps[:, 6, si * 128:si * 128 + 4 * KD]),
                  r=[("bk", 6)], w=[("col", si)])
            V(lambda: nc.vector.scalar_tensor_tensor(out=Gm, in0=colA[:, KD:2 * KD], scalar=1.0, in1=colA[:, 0:KD], op0=ALU.add, op1=ALU.mult),
              r=[("col", 0)], w=["Gs0"])
            V(lambda: nc.vector.scalar_tensor_tensor(out=cGm, in0=colA[:, 3 * KD:4 * KD], scalar=1.0, in1=colA[:, 0:KD], op0=ALU.add, op1=ALU.mult),
              r=[("col", 0)], w=["Gs1"])
            V(lambda: nc.vector.scalar_tensor_tensor(out=Gf, in0=colB[:, KD:2 * KD], scalar=1.0, in1=colB[:, 0:KD], op0=ALU.add, op1=ALU.mult),
              r=[("col", 1)], w=["Gs2"])
            S.flush()
            if stop == "P0":
                return nc

        def norm_tile(Tn, ps, src_ap, Gc, shc, dst, idx, xs2_dst=None, h2f=None, tpbanks=(4,)):
            r_ = idx % 2
            xt = Tn["xt"][r_]; ss = Tn["ss"]; rs = Tn["rs"]
            DM(lambda: nc.sync.dma_start(out=xt[:], in_=src_ap), w=[("xt", r_)])
            G(lambda: nc.gpsimd.memset(ss[:, r_:r_ + 1], 0.0), w=[("ss", r_)])
            A(lambda: nc.scalar.activation(out=Tn["junk"][:], in_=xt[:], func=AF.Square, scale=float(D) ** -0.5,
                                           accum_out=ss[:, r_:r_ + 1]), r=[("xt", r_), ("ss", r_)], w=["junk", ("ss", r_)])
            A(lambda: nc.scalar.activation(out=rs[:, r_:r_ + 1], in_=ss[:, r_:r_ + 1], func=AF.Sqrt, bias=EPS, scale=1.0),
              r=[("ss", r_)], w=[("rs", r_)])
            V(lambda: nc.vector.reciprocal(out=rs[:, r_:r_ + 1], in_=rs[:, r_:r_ + 1]), r=[("rs", r_)], w=[("rs", r_)])
            V(lambda: nc.vector.tensor_scalar(out=xt[:], in0=xt[:], scalar1=rs[:, r_:r_ + 1], scalar2=None, op0=ALU.mult),
              r=[("xt", r_), ("rs", r_)], w=[("xt", r_)])
            if xs2_dst is not None:
                xb = Tn["xb"]
                G(lambda: nc.gpsimd.tensor_copy(out=xb[:], in_=xt[:]), r=[("xt", r_)], w=["xb"])
                DM(lambda: nc.sync.dma_start(out=xs2_dst, in_=xb[:]), r=["xb"], w=["xs2_d"])
            for k in range(KD):
                if k % 4 == 0:
                    Tn["tq"][0] += 1
                tpbank = tpbanks[Tn["tq"][0] % len(tpbanks)]
                q = k % 4
                pa = ps[:, tpbank, q * 128:(q + 1) * 128]
                P(lambda k=k, pa=pa: nc.tensor.transpose(out=pa, in_=xt[:, k * 128:(k + 1) * 128], identity=ident),
                  r=[("xt", r_)], w=[("bk", tpbank)])
                d_ = dst(k)
                if h2f is None:
                    VA(lambda k=k, pa=pa, d_=d_: nc.vector.tensor_scalar(out=d_[0], in0=pa, scalar1=Gc[:, k:k + 1], scalar2=shc[:, k:k + 1],
                                                                         op0=ALU.mult, op1=ALU.add),
                       lambda k=k, pa=pa, d_=d_: nc.scalar.activation(out=d_[0], in_=pa, func=AF.Identity, scale=Gc[:, k:k + 1],
                                                                      bias=shc[:, k:k + 1]),
                       r=[("bk", tpbank)], w=[d_[1]])
                else:
                    h_ = h2f(k)
                    VA(lambda k=k, pa=pa, h_=h_: nc.vector.tensor_scalar(out=h_[0], in0=pa, scalar1=Gc[:, k:k + 1], scalar2=shc[:, k:k + 1],
                                                                         op0=ALU.mult, op1=ALU.add),
                       lambda k=k, pa=pa, h_=h_: nc.scalar.activation(out=h_[0], in_=pa, func=AF.Identity, scale=Gc[:, k:k + 1],
                                                                      bias=shc[:, k:k + 1]),
                       r=[("bk", tpbank)], w=[h_[1]])
                    G(lambda d_=d_, h_=h_: nc.gpsimd.tensor_copy(out=d_[0], in_=h_[0]), r=[h_[1]], w=[d_[1]])

        def norm_bufs(T, with_xb=False):
            Tn = {"xt": [T("xt0", [128, D]), T("xt1", [128, D])], "junk": T("junk", [128, D], BF16),
                  "ss": T("ss", [128, 2]), "rs": T("rs", [128, 2]), "tq": [0]}
            if with_xb: Tn["xb"] = T("xb", [128, D], BF16)
            return Tn

        def gates(Tg, ps, lo_ap, wg_b, bg_b, M1, key, zbanks=(6, 7), wkey="wg_b", bkey="bg_b"):
            sp = Tg["sp"]; Ek = Tg["Ek"]; zb = Tg["zb"]
            for hh in range(0, DK, 512):
                w_ = min(512, DK - hh); bank = zbanks[(hh // 512) % 2]
                P(lambda hh=hh, w_=w_, bank=bank: nc.tensor.matmul(out=ps[:, bank, 0:w_], lhsT=lo_ap, rhs=wg_b[:, hh:hh + w_], start=True, stop=True),
                  r=[key, wkey], w=[("zb", bank)])
                V(lambda hh=hh, w_=w_, bank=bank: nc.vector.tensor_tensor(out=zb[:, hh:hh + w_], in0=ps[:, bank, 0:w_], in1=bg_b[:, hh:hh + w_], op=ALU.add),
                  r=[("zb", bank), bkey], w=["zbuf"])
            A(lambda: nc.scalar.activation(out=zb[:], in_=zb[:], func=AF.Exp, scale=-1.0), r=["zbuf"], w=["zbuf"])
            A(lambda: nc.scalar.activation(out=sp[:], in_=zb[:], func=AF.Ln, bias=1.0, scale=1.0), r=["zbuf"], w=["sp"])
            for hh in range(0, DK, 512):
                w_ = min(512, DK - hh); bank = zbanks[(hh // 512) % 2]
                P(lambda hh=hh, w_=w_, bank=bank: nc.tensor.matmul(out=ps[:, bank, 0:w_], lhsT=M1, rhs=sp[:, hh:hh + w_], start=True, stop=True),
                  r=["sp"], w=[("zb", bank)])
                A(lambda hh=hh, w_=w_, bank=bank: nc.scalar.activation(out=Ek[:, hh:hh + w_], in_=ps[:, bank, 0:w_], func=AF.Exp),
                  r=[("zb", bank)], w=["Ek"])

        def state_update(Tg, ps, Sblk, khat, v_ap, ebL, skey, region, bkey, vkey="v_tm"):
            for p in range(NP):
                P(lambda p=p: nc.tensor.matmul(out=region, lhsT=khat[:, p * 128:(p + 1) * 128], rhs=v_ap(p), start=True, stop=True),
                  r=["khat", vkey], w=[bkey])
                for hb in range(2):
                    sl = Sblk[hb * 64:(hb + 1) * 64, p * 256 + hb * 128:p * 256 + (hb + 1) * 128]
                    V(lambda p=p, hb=hb, sl=sl: nc.vector.scalar_tensor_tensor(
                        out=sl, in0=sl, scalar=ebL[hb * 64:(hb + 1) * 64, p:p + 1],
                        in1=region[hb * 64:(hb + 1) * 64, hb * 128:(hb + 1) * 128], op0=ALU.mult, op1=ALU.add),
                      r=[bkey, "ebL", skey], w=[skey])

        def load_cast_w(Tw, src_fn, nk, ncols, key):
            r_ = Tw["wi"][0] % len(Tw["wb"]); Tw["wi"][0] += 1
            wb = Tw["wb"][r_]
            for k0 in range(0, nk, KP):
                k1 = min(nk, k0 + KP)
                s_ = Tw["si"][0] % len(Tw["st"]); Tw["si"][0] += 1
                stg = Tw["st"][s_]
                DM(lambda k0=k0, k1=k1, stg=stg: nc.sync.dma_start(out=stg[:, 0:k1 - k0, 0:ncols],
                                                                   in_=src_fn(k0, k1).rearrange("(k p) n -> p k n", p=128)),
                   w=[("wstg", key, s_)])
                if Tw["si"][0] % 2:
                    V(lambda k0=k0, k1=k1, stg=stg, wb=wb: nc.vector.tensor_copy(out=wb[:, k0:k1, 0:ncols], in_=stg[:, 0:k1 - k0, 0:ncols]),
                      r=[("wstg", key, s_)], w=[("wb", key, r_)])
                else:
                    A(lambda k0=k0, k1=k1, stg=stg, wb=wb: nc.scalar.copy(out=wb[:, k0:k1, 0:ncols], in_=stg[:, 0:k1 - k0, 0:ncols]),
                      r=[("wstg", key, s_)], w=[("wb", key, r_)])
            return wb, ("wb", key, r_)

        with contextlib.ExitStack() as ph:
            T = lambda n, s, d=F32: ph.enter_context(nc.sbuf_tensor(uname(n), list(s), d))
            ps = ph.enter_context(nc.psum_tensor("psA", [128, 8, 512], F32))
            Tn = norm_bufs(T)
            TPA = min(2, NT); NTA = TPA * 128
            hT = [T("hT%d" % i, [128, KD, NTA], BF16) for i in range(2)]
            k_tm = [T("k_tm%d" % i, [128, TPA, DK], BF16) for i in range(2)]
            v_tm = [T("v_tm%d" % i, [128, TPA, DG], BF16) for i in range(2)]
            loA = [T("loA%d" % i, [16, NTA], BF16) for i in range(2)]
            stA = [T("stA%d" % i, [128, KP, 256]) for i in range(2)]
            wbA = [T("wbA%d" % i, [128, KP, 256], BF16) for i in range(3)]
            wgsf = T("wgsf", [128, KD, 16]); wguf = T("wguf", [16, DK])
            wgsb = [T("wgsb%d" % i, [128, KD, 16], BF16) for i in range(2)]
            wgub = [T("wgub%d" % i, [16, DK], BF16) for i in range(2)]
            bgb = [T("bgb%d" % i, [128, DK]) for i in range(2)]
            Tg = {"sp": T("sp", [128, DK]), "Ek": T("Ek", [128, DK]), "zb": T("zb", [128, DK])}
            khat = T("khat", [128, DK], BF16); ebL = T("ebL", [128, NP])
            Sb = T("Sb", [128, NP * 256]); Sctxb = T("Sctxb", [128, NP * 256])
            si_ = [0]; stc = [0]; first_pass = [True]

            slots = [(lambda t: ctx2[c.CTX + t * 128:c.CTX + (t + 1) * 128, :], c.CT, cGm, cshm, 1, "ctxb"),
                     (lambda t: ctx2[t * 128:(t + 1) * 128, :], c.CT, cGm, cshm, 0, "ctxf")]
            for b_ in range(c.NS):
                slots.append((lambda t, b_=b_: x_oth[b_ * TOK + t * 128:b_ * TOK + (t + 1) * 128, :], NT, Gm, shm, 2 + b_, ("oth", b_)))
            steps = []
            for sl_i, (srcf, ntl, Gc_, shc_, sidx, tag) in enumerate(slots):
                for h0 in range(0, ntl, TPA):
                    steps.append(dict(src=srcf, h0=h0, nth=min(TPA, ntl - h0), Gc=Gc_, shc=shc_, sidx=sidx, tag=tag, sp=sl_i % 2,
                                      first=(h0 == 0), last=(h0 + TPA >= ntl)))

            def load_slot_weights(st):
                sidx, sp_ = st["sidx"], st["sp"]
                DM(lambda: nc.sync.dma_start(out=wgsf[:], in_=w_gs[sidx * D:(sidx + 1) * D, :].rearrange("(k p) n -> p k n", p=128)), w=["wgsf"])
                V(lambda: nc.vector.tensor_copy(out=wgsb[sp_][:], in_=wgsf[:]), r=["wgsf"], w=[("wgsb", sp_)])
                DM(lambda: nc.sync.dma_start(out=wguf[:], in_=wgu_s[sidx * 16:(sidx + 1) * 16, :]), w=["wguf"])
                V(lambda: nc.vector.tensor_copy(out=wgub[sp_][:], in_=wguf[:]), r=["wguf"], w=[("wg_b", sp_)])
                DM(lambda: nc.sync.dma_start(out=bgb[sp_][:], in_=bgu_s[sidx].partition_broadcast(128)), w=[("bg_b", sp_)])

            def front(st, pb):
                if st["first"]:
                    load_slot_weights(st)
                nth = st["nth"]
                for j in range(nth):
                    norm_tile(Tn, ps, st["src"](st["h0"] + j), st["Gc"], st["shc"],
                              lambda k, j=j, pb=pb: (hT[pb][:, k, j * 128:(j + 1) * 128], ("hT", pb, k, j)), si_[0], tpbanks=(4,))
                    si_[0] += 1
                for k in range(KD):
                    P(lambda k=k, nth=nth, pb=pb, sp_=st["sp"]: nc.tensor.matmul(out=ps[0:16, 5, 0:nth * 128], lhsT=wgsb[sp_][:, k, :], rhs=hT[pb][:, k, 0:nth * 128],
                                                                              start=(k == 0), stop=(k == KD - 1)),
                      r=[("wgsb", st["sp"])] + [("hT", pb, k, j) for j in range(nth)], w=[("bk", 5)])
                V(lambda nth=nth, pb=pb: nc.vector.tensor_copy(out=loA[pb][:, 0:nth * 128], in_=ps[0:16, 5, 0:nth * 128]), r=[("bk", 5)], w=[("loA", pb)])

            def proj_blocks(st, pb):
                nth = st["nth"]
                blocks = [(w_k, c0, min(256, DK - c0), k_tm[pb], "k") for c0 in range(0, DK, 256)] + \
                         [(w_v, c0, min(256, DG - c0), v_tm[pb], "v") for c0 in range(0, DG, 256)]
                outl = []
                for bi, (wsrc, c0, bw, dst, nm) in enumerate(blocks):
                    def emit(bi=bi, wsrc=wsrc, c0=c0, bw=bw, dst=dst, nm=nm):
                        bofs = (bi % 2) * 2
                        for k0 in range(0, KD, KP):
                            s_ = stc[0] % 3; stc[0] += 1
                            crow = (bi * (KD // KP) + k0 // KP) * 128
                            if first_pass[0]:
                                g_ = s_ % 2
                                DM(lambda g_=g_, k0=k0: nc.sync.dma_start(
                                    out=stA[g_][:, :, 0:bw], in_=wsrc[k0 * 128:(k0 + KP) * 128, c0:c0 + bw].rearrange("(k p) n -> p k n", p=128)),
                                   w=[("stA", g_)])
                                VA(lambda s_=s_, g_=g_: nc.vector.tensor_copy(out=wbA[s_][:, :, 0:bw], in_=stA[g_][:, :, 0:bw]),
                                   lambda s_=s_, g_=g_: nc.scalar.copy(out=wbA[s_][:, :, 0:bw], in_=stA[g_][:, :, 0:bw]),
                                   r=[("stA", g_)], w=[("wbA", s_)])
                                DM(lambda s_=s_, crow=crow: nc.sync.dma_start(out=wkv_c[crow:crow + 128, :], in_=wbA[s_][:].rearrange("p k n -> p (k n)")),
                                   r=[("wbA", s_)], w=[("wkvc", crow)])
                            else:
                                DM(lambda s_=s_, crow=crow: nc.sync.dma_start(out=wbA[s_][:].rearrange("p k n -> p (k n)"), in_=wkv_c[crow:crow + 128, :]),
                                   r=[("wkvc", crow)], w=[("wbA", s_)])
                            for j in range(nth):
                                for kk in range(KP):
                                    k = k0 + kk
                                    P(lambda j=j, k=k, kk=kk, s_=s_: nc.tensor.matmul(
                                        out=ps[:, bofs + j, 0:bw], lhsT=hT[pb][:, k, j * 128:(j + 1) * 128], rhs=wbA[s_][:, kk, 0:bw],
                                        start=(k == 0), stop=(k == KD - 1)), r=[("wbA", s_), ("hT", pb, k, j)], w=[("bk", bofs + j)])
                        for j in range(nth):
                            VA(lambda j=j: nc.vector.tensor_copy(out=dst[:, j, c0:c0 + bw], in_=ps[:, bofs + j, 0:bw]),
                               lambda j=j: nc.scalar.copy(out=dst[:, j, c0:c0 + bw], in_=ps[:, bofs + j, 0:bw]),
                               r=[("bk", bofs + j)], w=[(nm + "_tm", pb)])
                    outl.append(emit)
                return outl

            def boundary(b):
                fo = off["flags"][0] + 3 * b
                V(lambda: nc.vector.scalar_tensor_tensor(out=S0[:, 0, :], in0=Sb[:], scalar=cm[:, fo:fo + 1], in1=S0[:, 0, :],
                                                         op0=ALU.mult, op1=ALU.add), r=["Sb", "S0f"], w=["S0f"])
                for hf in range(2):
                    cs_ = slice(hf * DK, (hf + 1) * DK)
                    V(lambda cs_=cs_: nc.vector.tensor_scalar(out=Tg["zb"][:], in0=Sctxb[:, cs_], scalar1=cm[:, fo + 2:fo + 3], scalar2=None, op0=ALU.mult),
                      r=["Sctxb", "zbuf"], w=["zbuf"])
                    V(lambda cs_=cs_: nc.vector.scalar_tensor_tensor(out=Sb[:, cs_], in0=Sb[:, cs_], scalar=cm[:, fo + 1:fo + 2], in1=Tg["zb"][:],
                                                                     op0=ALU.mult, op1=ALU.add), r=["Sb", "zbuf", "S0f"], w=["Sb"])

            def rec_micro(st, pb):
                ms = []
                sp_ = st["sp"]
                if st["first"] and st["tag"] == "ctxb":
                    ms.append(lambda: V(lambda: nc.vector.memset(Sb[:], 0.0), w=["Sb"]))
                for j in range(st["nth"]):
                    def m1(j=j):
                        gates(Tg, ps, loA[pb][:, j * 128:(j + 1) * 128], wgub[sp_], bgb[sp_], CM("M1F"), ("loA", pb), wkey=("wg_b", sp_), bkey=("bg_b", sp_))
                    def m2(j=j):
                        V(lambda: nc.vector.tensor_tensor(out=khat[:], in0=k_tm[pb][:, j, :], in1=Tg["Ek"][:], op=ALU.mult),
                          r=[("k_tm", pb), "Ek"], w=["khat"])
                        for p in range(NP):
                            P(lambda p=p: nc.tensor.matmul(out=ps[:, 5, 480 + p:481 + p], lhsT=Tg["sp"][:, p * 128:(p + 1) * 128],
                                                           rhs=CM("negcol"), start=True, stop=True), r=["sp"], w=[("bk", 5)])
                        A(lambda: nc.scalar.activation(out=ebL[:], in_=ps[:, 5, 480:480 + NP], func=AF.Exp), r=[("bk", 5)], w=["ebL"])
                    def m3(j=j):
                        state_update(Tg, ps, Sb, khat, lambda p: v_tm[pb][:, j, p * 256:(p + 1) * 256], ebL, "Sb", ps[:, 4, 256:512], ("bk", 4), vkey=("v_tm", pb))
                    ms += [m1, m2, m3]
                if st["last"]:
                    tag = st["tag"]
                    if tag == "ctxb":
                        def post():
                            V(lambda: nc.vector.tensor_copy(out=Sctxb[:], in_=Sb[:]), r=["Sb"], w=["Sctxb"])
                            V(lambda: nc.vector.memset(Sb[:], 0.0), r=["Sctxb"], w=["Sb"])
                            V(lambda: nc.vector.memset(S0[:, 0, :], 0.0), w=["S0f"])
                    elif tag == "ctxf":
                        def post():
                            boundary(0)
                    else:
                        def post(b_=tag[1]):
                            boundary(b_ + 1)
                    ms.append(post)
                return ms

            if c.NS == 0:
                pass
            n_st = len(steps)
            front(steps[0], 0)
            for em in proj_blocks(steps[0], 0):
                em()
            first_pass[0] = False
            for i in range(n_st):
                pb = i % 2
                blocks = []
                if i + 1 < n_st:
                    front(steps[i + 1], 1 - pb)
                    blocks = proj_blocks(steps[i + 1], 1 - pb)
                micro = rec_micro(steps[i], pb)
                nb_, nm_ = len(blocks), len(micro)
                mi = 0
                for bi in range(nb_):
                    blocks[bi]()
                    want = ((bi + 1) * nm_) // nb_
                    while mi < want:
                        micro[mi](); mi += 1
                while mi < nm_:
                    micro[mi](); mi += 1
            V(lambda: nc.vector.tensor_copy(out=S0[:, 1, :], in_=Sb[:]), r=["Sb"], w=["S0b"])
            if dbg:
                dS = nc.dram_tensor("dbg_S0", [128, 2 * NP * 256], F32, kind="ExternalOutput").ap()
                DM(lambda: nc.sync.dma_start(out=dS, in_=S0[:].rearrange("p a n -> p (a n)")), r=["S0f", "S0b"])
            S.flush()
            if stop == "A":
                return nc

        with contextlib.ExitStack() as ph:
            T = lambda n, s, d=F32: ph.enter_context(nc.sbuf_tensor(uname(n), list(s), d))
            ps = ph.enter_context(nc.psum_tensor("psB", [128, 8, 512], F32))
            Tn = norm_bufs(T)
            hT = T("hTo", [128, KD, TOK], BF16)
            Tw = {"st": [T("stB%d" % i, [128, KP, 256]) for i in range(3)], "wb": [T("wbB%d" % i, [128, KD, 256], BF16) for i in range(2)],
                  "wi": [0], "si": [0]}
            evb = [T("evb%d" % i, [128, 512], BF16) for i in range(4)]
            evf = [T("evf%d" % i, [128, 512]) for i in range(2)]
            ec = [0]; bk = [0]
            import os
            lvl = int(os.environ.get('KDBG_B2', '9'))
            for t in range(NT if lvl >= 0 else 0):
                norm_tile(Tn, ps, x_own[t * 128:(t + 1) * 128, :], Gm, shm,
                          lambda k, t=t: (hT[:, k, t * 128:(t + 1) * 128], ("hT", k, t)), t, tpbanks=(6, 7))
            hk_all = lambda k: [("hT", k, t) for t in range(NT)]

            def nbank():
                b = bk[0] % 6; bk[0] += 1
                return b

            def fm_group(wb, wkey, c_lo, c_hi, tb, bank, rows=128):
                for k in range(KD):
                    P(lambda k=k: nc.tensor.matmul(out=ps[0:rows, bank, 0:NTB], lhsT=wb[:, k, c_lo:c_hi], rhs=hT[:, k, tb * NTB:(tb + 1) * NTB],
                                                   start=(k == 0), stop=(k == KD - 1)), r=[wkey] + hk_all(k), w=[("pb", bank)])

            def tm_group(wb, wkey, bw, t, bank):
                for k in range(KD):
                    P(lambda k=k: nc.tensor.matmul(out=ps[:, bank, 0:bw], lhsT=hT[:, k, t * 128:(t + 1) * 128], rhs=wb[:, k, 0:bw],
                                                   start=(k == 0), stop=(k == KD - 1)), r=[wkey, ("hT", k, t)], w=[("pb", bank)])

            def evac_bf(bank, n, dram_ap, scale=None, func=None):
                e_ = ec[0] % 4; ec[0] += 1
                if func is not None:
                    A(lambda: nc.scalar.activation(out=evb[e_][:, 0:n], in_=ps[:, bank, 0:n], func=func), r=[("pb", bank)], w=[("evb", e_)])
                elif scale is not None:
                    A(lambda: nc.scalar.mul(out=evb[e_][:, 0:n], in_=ps[:, bank, 0:n], mul=scale), r=[("pb", bank)], w=[("evb", e_)])
                else:
                    VA(lambda: nc.vector.tensor_copy(out=evb[e_][:, 0:n], in_=ps[:, bank, 0:n]),
                       lambda: nc.scalar.copy(out=evb[e_][:, 0:n], in_=ps[:, bank, 0:n]), r=[("pb", bank)], w=[("evb", e_)])
                DM(lambda: nc.sync.dma_start(out=dram_ap, in_=evb[e_][:, 0:n]), r=[("evb", e_)], w=["scr"])

            for (wsrc, dstT, scl) in ((w_q, qT_d, 0.125), (w_k, kT_d, None))[:max(0, lvl)]:
                for c0 in range(0, DK, 256):
                    bw = min(256, DK - c0)
                    wb, wkey = load_cast_w(Tw, lambda k0, k1, wsrc=wsrc, c0=c0, bw=bw: wsrc[k0 * 128:k1 * 128, c0:c0 + bw], KD, bw, "B")
                    for sub in range(bw // 128):
                        for tb in range(TOK // NTB):
                            bank = nbank()
                            fm_group(wb, wkey, sub * 128, (sub + 1) * 128, tb, bank)
                            evac_bf(bank, NTB, dstT[c0 + sub * 128:c0 + (sub + 1) * 128, tb * NTB:(tb + 1) * NTB], scale=scl)
                    if wsrc is w_k:
                        for t in range(NT):
                            bank = nbank()
                            tm_group(wb, wkey, bw, t, bank)
                            evac_bf(bank, bw, k_d[t * 128:(t + 1) * 128, c0:c0 + bw])
            for (wsrc, dst, fn) in ((w_v, v_d, None), (w_r, sr_d, AF.Silu))[:max(0, lvl - 2)]:
                for c0 in range(0, DG, 256):
                    bw = min(256, DG - c0)
                    wb, wkey = load_cast_w(Tw, lambda k0, k1, wsrc=wsrc, c0=c0, bw=bw: wsrc[k0 * 128:k1 * 128, c0:c0 + bw], KD, bw, "B")
                    for t in range(NT):
                        bank = nbank()
                        tm_group(wb, wkey, bw, t, bank)
                        evac_bf(bank, bw, dst[t * 128:(t + 1) * 128, c0:c0 + bw], func=fn)
            for cch in range(NCC if lvl >= 5 else 0):
                wb, wkey = load_cast_w(Tw, lambda k0, k1, cch=cch: w_ab[k0 * 128:k1 * 128, cch * 256:(cch + 1) * 256], KD, 256, "B")
                for tb in range(TOK // NTB):
                    ba = nbank(); fm_group(wb, wkey, 0, 128, tb, ba)
                    bb = nbank(); fm_group(wb, wkey, 128, 256, tb, bb)
                    f_ = ec[0] % 2; e_ = ec[0] % 4; ec[0] += 1
                    A(lambda bb=bb, f_=f_: nc.scalar.activation(out=evf[f_][:, 0:NTB], in_=ps[:, bb, 0:NTB], func=AF.Sigmoid),
                      r=[("pb", bb)], w=[("evf", f_)])
                    V(lambda ba=ba, f_=f_, e_=e_: nc.vector.tensor_tensor(out=evb[e_][:, 0:NTB], in0=ps[:, ba, 0:NTB], in1=evf[f_][:, 0:NTB], op=ALU.mult),
                      r=[("pb", ba), ("evf", f_)], w=[("evb", e_)])
                    DM(lambda cch=cch, tb=tb, e_=e_: nc.sync.dma_start(out=u_d[cch * 128:(cch + 1) * 128, tb * NTB:(tb + 1) * NTB], in_=evb[e_][:, 0:NTB]),
                       r=[("evb", e_)], w=["scr"])
            if lvl >= 6:
                wb, wkey = load_cast_w(Tw, lambda k0, k1: w_g[k0 * 128:k1 * 128, 0:32], KD, 32, "B")
            for d_ in range(2 if lvl >= 6 else 0):
                for tb in range(TOK // NTB):
                    bank = nbank()
                    fm_group(wb, wkey, d_ * 16, (d_ + 1) * 16, tb, bank, rows=16)
                    V(lambda d_=d_, tb=tb, bank=bank: nc.vector.tensor_copy(out=lowT[:, d_, tb * NTB:(tb + 1) * NTB], in_=ps[0:16, bank, 0:NTB]),
                      r=[("pb", bank)], w=["lowT"])
            S.flush()
            if stop == "B2":
                return nc

        own = contextlib.ExitStack()
        with own:
            actT = own.enter_context(nc.sbuf_tensor("s_actT", [128, KD, TOK], BF16))
            with contextlib.ExitStack() as ph:
                T = lambda n, s, d=F32: ph.enter_context(nc.sbuf_tensor(uname(n), list(s), d))
                ps = ph.enter_context(nc.psum_tensor("psG", [128, 8, 512], F32))
                kt = [T("kt%d" % i, [128, DK], BF16) for i in range(2)]
                vt = [T("vt%d" % i, [128, DG], BF16) for i in range(2)]
                srt = [T("srt%d" % i, [128, DG], BF16) for i in range(2)]
                obt = [T("obt%d" % i, [128, DG]) for i in range(2)]
                qTt = [T("qTt%d" % i, [128, NP, 128], BF16) for i in range(2)]
                kTt = [T("kTt%d" % i, [128, NP, 128], BF16) for i in range(2)]
                wgub = T("wgub2", [16, 2, DK], BF16); bgb = T("bgb2", [128, 2, DK])
                Tg = {"sp": T("sp2", [128, DK]), "Ek": T("Ek2", [128, DK]), "zb": T("zb2", [128, DK])}
                khat = T("khat2", [128, DK], BF16)
                Eb = [T("Eb%d" % i, [128, 128]) for i in range(2)]; Enb = [T("Enb%d" % i, [128, 128]) for i in range(2)]
                qd = [T("qd%d" % i, [128, 128], BF16) for i in range(2)]; kd = [T("kd%d" % i, [128, 128], BF16) for i in range(2)]
                Qblk = [T("Qblk%d" % i, [128, 256], BF16) for i in range(2)]
                att = [T("att%d" % i, [128, 256], BF16) for i in range(2)]
                Sw = T("Sw", [128, NP * 256]); Sbf = T("Sbf", [128, NP * 256], BF16)
                obuf = obt[0]; osum = [T("osum%d" % i, [128, 256]) for i in range(2)]
                gsb = [T("gsb%d" % i, [128, 256]) for i in range(2)]; onb = [T("onb%d" % i, [128, 256]) for i in range(2)]
                ggb = T("ggb", [128, DG]); ssh = T("ssh", [128, 4]); rsh = T("rsh", [128, 4]); junk2 = T("junk2", [128, 128])
                for d_ in range(2):
                    DM(lambda d_=d_: nc.sync.dma_start(out=Tg["zb"][0:16, :], in_=wgu[d_ * 16:(d_ + 1) * 16, :]), w=["zbuf"])
                    V(lambda d_=d_: nc.vector.tensor_copy(out=wgub[:, d_, :], in_=Tg["zb"][0:16, :]), r=["zbuf"], w=["wg_b"])
                for d_ in range(2):
                    DM(lambda d_=d_: nc.sync.dma_start(out=bgb[:, d_, :], in_=bgu[d_].partition_broadcast(128)), w=["bg_b"])
                DM(lambda: nc.sync.dma_start(out=ggb[:], in_=g_gla.partition_broadcast(128)), w=["ggb"])
                for i in range(2):
                    V(lambda i=i: nc.vector.memset(Qblk[i][:], 0.0), w=[("Qblk", i)])
                uc = [0]
                for dirn in (1, 0):
                    Mc = CM("McB") if dirn else CM("McF"); M1 = CM("M1B") if dirn else CM("M1F")
                    mask2 = CM("maskB2") if dirn else CM("maskF2")
                    V(lambda dirn=dirn: nc.vector.tensor_copy(out=Sw[:], in_=S0[:, dirn, :]), r=["Sw", "Sbf"], w=["Sw"])
                    G(lambda: nc.gpsimd.tensor_copy(out=Sbf[:], in_=Sw[:]), r=["Sw"], w=["Sbf"])
                    order = range(NT - 1, -1, -1) if dirn else range(NT)
                    for ti, t in enumerate(order):
                        r_ = ti % 2
                        rows = slice(t * 128, (t + 1) * 128)
                        DM(lambda r_=r_, rows=rows: nc.sync.dma_start(out=kt[r_][:], in_=k_d[rows, :]), w=[("kt", r_)])
                        DM(lambda r_=r_, rows=rows: nc.sync.dma_start(out=vt[r_][:], in_=v_d[rows, :]), w=[("vt", r_)])
                        DM(lambda r_=r_, rows=rows: nc.sync.dma_start(out=qTt[r_][:], in_=qT_d.rearrange("(p r) t -> r p t", r=128)[:, :, rows]),
                           w=[("qTt", r_)])
                        DM(lambda r_=r_, rows=rows: nc.sync.dma_start(out=kTt[r_][:], in_=kT_d.rearrange("(p r) t -> r p t", r=128)[:, :, rows]),
                           w=[("kTt", r_)])
                        if not dirn:
                            DM(lambda r_=r_, rows=rows: nc.sync.dma_start(out=srt[r_][:], in_=sr_d[rows, :]), w=[("srt", r_)])
                            DM(lambda r_=r_, rows=rows: nc.sync.dma_start(out=obt[r_][:], in_=ob_d[rows, :]), r=[("ob_d", t), "obuf"], w=[("obt", r_), "obuf"])
                        gates(Tg, ps, lowT[:, dirn, rows], wgub[:, dirn, :], bgb[:, dirn, :], M1, "lowT")
                        V(lambda r_=r_: nc.vector.tensor_tensor(out=khat[:], in0=kt[r_][:], in1=Tg["Ek"][:], op=ALU.mult),
                          r=[("kt", r_), "Ek"], w=["khat"])
                        for p in range(NP):
                            u_ = uc[0] % 2; uc[0] += 1
                            q4 = 0
                            bTp = ps[:, 0, 0:128]
                            P(lambda p=p, bTp=bTp, Mc=Mc: nc.tensor.matmul(out=bTp, lhsT=Tg["sp"][:, p * 128:(p + 1) * 128], rhs=Mc, start=True, stop=True),
                              r=["sp"], w=[("bk", 0)])
                            A(lambda u_=u_, bTp=bTp: nc.scalar.activation(out=Eb[u_][:], in_=bTp, func=AF.Exp), r=[("bk", 0)], w=[("Eb", u_)])
                            A(lambda u_=u_, bTp=bTp: nc.scalar.activation(out=Enb[u_][:], in_=bTp, func=AF.Exp, scale=-1.0), r=[("bk", 0)], w=[("Enb", u_)])
                            V(lambda p=p, u_=u_, r_=r_: nc.vector.tensor_tensor(out=qd[u_][:], in0=qTt[r_][:, p, :], in1=Eb[u_][:], op=ALU.mult),
                              r=[("qTt", r_), ("Eb", u_)], w=[("qd", u_)])
                            G(lambda p=p, u_=u_, r_=r_: nc.gpsimd.tensor_tensor(out=kd[u_][:], in0=kTt[r_][:, p, :], in1=Enb[u_][:], op=ALU.mult),
                              r=[("kTt", r_), ("Enb", u_)], w=[("kd", u_)])
                            for hb in range(2):
                                G(lambda u_=u_, hb=hb: nc.gpsimd.tensor_copy(out=Qblk[u_][hb * 64:(hb + 1) * 64, hb * 128:(hb + 1) * 128],
                                                                             in_=qd[u_][hb * 64:(hb + 1) * 64, :]), r=[("qd", u_)], w=[("Qblk", u_)])
                            ap_ = ps[:, 1 + u_, 0:256]
                            P(lambda u_=u_, ap_=ap_: nc.tensor.matmul(out=ap_, lhsT=kd[u_][:], rhs=Qblk[u_][:], start=True, stop=True),
                              r=[("kd", u_), ("Qblk", u_)], w=[("bk", 1 + u_)])
                            V(lambda u_=u_, ap_=ap_, mask2=mask2: nc.vector.tensor_tensor(out=att[u_][:], in0=ap_, in1=mask2, op=ALU.mult),
                              r=[("bk", 1 + u_)], w=[("att", u_)])
                            op_ = ps[:, 3 + u_, 0:256]
                            P(lambda p=p, u_=u_, op_=op_: nc.tensor.matmul(out=op_, lhsT=qd[u_][:], rhs=Sbf[:, p * 256:(p + 1) * 256], start=True, stop=False),
                              r=[("qd", u_), "Sbf"], w=[("bk", 3 + u_)])
                            for hb in range(2):
                                P(lambda p=p, u_=u_, hb=hb, r_=r_: nc.tensor.matmul(
                                    out=ps[:, 3 + u_, hb * 128:(hb + 1) * 128], lhsT=att[u_][:, hb * 128:(hb + 1) * 128],
                                    rhs=vt[r_][:, p * 256 + hb * 128:p * 256 + (hb + 1) * 128], start=False, stop=(hb == 1)),
                                  r=[("att", u_), ("vt", r_)], w=[("bk", 3 + u_)])
                            ecol = 0 if dirn else 127
                            P(lambda p=p, r_=r_: nc.tensor.matmul(out=ps[:, 5, 0:256], lhsT=khat[:, p * 128:(p + 1) * 128],
                                                                   rhs=vt[r_][:, p * 256:(p + 1) * 256], start=True, stop=True),
                              r=["khat", ("vt", r_)], w=[("bk", 5)])
                            for hb in range(2):
                                sl = Sw[hb * 64:(hb + 1) * 64, p * 256 + hb * 128:p * 256 + (hb + 1) * 128]
                                V(lambda hb=hb, sl=sl, u_=u_, ecol=ecol: nc.vector.scalar_tensor_tensor(
                                    out=sl, in0=sl, scalar=Eb[u_][hb * 64:(hb + 1) * 64, ecol:ecol + 1],
                                    in1=ps[hb * 64:(hb + 1) * 64, 5, hb * 128:(hb + 1) * 128], op0=ALU.mult, op1=ALU.add),
                                  r=[("bk", 5), ("Eb", u_), "Sw", "Sbf", ("bk", 3 + u_)], w=["Sw"])
                            G(lambda p=p: nc.gpsimd.tensor_copy(out=Sbf[:, p * 256:(p + 1) * 256], in_=Sw[:, p * 256:(p + 1) * 256]),
                              r=["Sw"], w=["Sbf"])
                            if dirn:
                                VA(lambda p=p, op_=op_: nc.vector.tensor_copy(out=obuf[:, p * 256:(p + 1) * 256], in_=op_),
                                   lambda p=p, op_=op_: nc.scalar.copy(out=obuf[:, p * 256:(p + 1) * 256], in_=op_), r=[("bk", 3 + u_)], w=["obuf"])
                            else:
                                V(lambda p=p, u_=u_, op_=op_, r_=r_: nc.vector.tensor_tensor(out=osum[u_][:], in0=op_, in1=obt[r_][:, p * 256:(p + 1) * 256], op=ALU.add),
                                  r=[("bk", 3 + u_), ("obt", r_)], w=[("osum", u_)])
                                G(lambda u_=u_: nc.gpsimd.memset(ssh[:, 2 * u_:2 * u_ + 2], 0.0), w=[("ssh", u_)])
                                for hb in range(2):
                                    A(lambda u_=u_, hb=hb: nc.scalar.activation(out=junk2[:], in_=osum[u_][:, hb * 128:(hb + 1) * 128], func=AF.Square,
                                                                                scale=128.0 ** -0.5, accum_out=ssh[:, 2 * u_ + hb:2 * u_ + hb + 1]),
                                      r=[("osum", u_), ("ssh", u_)], w=[("ssh", u_), "junk2"])
                                A(lambda u_=u_: nc.scalar.activation(out=rsh[:, 2 * u_:2 * u_ + 2], in_=ssh[:, 2 * u_:2 * u_ + 2], func=AF.Sqrt, bias=EPS, scale=1.0),
                                  r=[("ssh", u_)], w=[("rsh", u_)])
                                V(lambda u_=u_: nc.vector.reciprocal(out=rsh[:, 2 * u_:2 * u_ + 2], in_=rsh[:, 2 * u_:2 * u_ + 2]), r=[("rsh", u_)], w=[("rsh", u_)])
                                G(lambda p=p, u_=u_, r_=r_: nc.gpsimd.tensor_tensor(out=gsb[u_][:], in0=ggb[:, p * 256:(p + 1) * 256], in1=srt[r_][:, p * 256:(p + 1) * 256], op=ALU.mult),
                                  r=["ggb", ("srt", r_)], w=[("gsb", u_)])
                                for hb in range(2):
                                    V(lambda u_=u_, hb=hb: nc.vector.scalar_tensor_tensor(
                                        out=onb[u_][:, hb * 128:(hb + 1) * 128], in0=osum[u_][:, hb * 128:(hb + 1) * 128],
                                        scalar=rsh[:, 2 * u_ + hb:2 * u_ + hb + 1], in1=gsb[u_][:, hb * 128:(hb + 1) * 128], op0=ALU.mult, op1=ALU.mult),
                                      r=[("osum", u_), ("rsh", u_), ("gsb", u_)], w=[("onb", u_)])
                                for hb in range(2):
                                    tp = ps[:, 5, 256 + hb * 128:256 + (hb + 1) * 128]
                                    P(lambda u_=u_, hb=hb, tp=tp: nc.tensor.transpose(out=tp, in_=onb[u_][:, hb * 128:(hb + 1) * 128], identity=ident),
                                      r=[("onb", u_)], w=[("bk", 5)])
                                    VA(lambda p=p, hb=hb, tp=tp, rows=rows: nc.vector.tensor_copy(out=actT[:, 2 * p + hb, rows], in_=tp),
                                       lambda p=p, hb=hb, tp=tp, rows=rows: nc.scalar.copy(out=actT[:, 2 * p + hb, rows], in_=tp),
                                       r=[("bk", 5)], w=[("actT", 2 * p + hb)])
                        if dirn:
                            DM(lambda rows=rows: nc.sync.dma_start(out=ob_d[rows, :], in_=obuf[:]), r=["obuf"], w=[("ob_d", t)])
                S.flush()
                if stop == "B3":
                    return nc

            with contextlib.ExitStack() as ph:
                T = lambda n, s, d=F32: ph.enter_context(nc.sbuf_tensor(uname(n), list(s), d))
                ps = ph.enter_context(nc.psum_tensor("psC", [128, 8, 512], F32))
                ybuf = T("ybuf", [128, NCC, TOK])
                ub = [T("ub%d" % i, [128, TOK], BF16) for i in range(2)]
                ysq = [T("ysq%d" % i, [128, TOK]) for i in range(2)]
                wrow = T("wrow", [32, DC]); crow = T("crow", [4, DC])
                wdw = T("wdw", [128, NCC, 32]); cv = T("cv", [128, NCC, 4])
                mu = T("mu", [128, TOK]); rsv = T("rsv", [128, TOK]); t1 = [T("t1_%d" % i, [128, TOK]) for i in range(2)]
                V(lambda: nc.vector.memset(wrow[:], 0.0), w=["wrow"])
                V(lambda: nc.vector.memset(crow[:], 0.0), w=["crow"])
                DM(lambda: nc.sync.dma_start(out=wrow[0:CONV_W, :], in_=w_dw), w=["wrow"])
                DM(lambda: nc.sync.dma_start(out=crow[0:3, :], in_=cvec), w=["crow"])
                for cch in range(NCC):
                    q = cch % 2
                    P(lambda cch=cch, q=q: nc.tensor.transpose(out=ps[:, 4, q * 64:q * 64 + 32], in_=wrow[:, cch * 128:(cch + 1) * 128], identity=ident[0:32, 0:32]),
                      r=["wrow"], w=[("bk", 4)])
                    V(lambda cch=cch, q=q: nc.vector.tensor_copy(out=wdw[:, cch, :], in_=ps[:, 4, q * 64:q * 64 + 32]), r=[("bk", 4)], w=["wdw"])
                    P(lambda cch=cch, q=q: nc.tensor.transpose(out=ps[:, 4, 256 + q * 64:256 + q * 64 + 4], in_=crow[:, cch * 128:(cch + 1) * 128], identity=ident[0:4, 0:4]),
                      r=["crow"], w=[("bk", 4)])
                    V(lambda cch=cch, q=q: nc.vector.tensor_copy(out=cv[:, cch, :], in_=ps[:, 4, 256 + q * 64:256 + q * 64 + 4]), r=[("bk", 4)], w=["cv"])
                GW = c.GRID_W
                NH2 = TOK // NTB
                for c0 in range(0, NCC, 2):
                    cs = [cc_ for cc_ in (c0, c0 + 1) if cc_ < NCC]
                    for cc_ in cs:
                        DM(lambda cc_=cc_: nc.sync.dma_start(out=ub[cc_ % 2][:], in_=u_d[cc_ * 128:(cc_ + 1) * 128, :]), w=[("ub", cc_ % 2)])
                    for j in [15] + [j for j in range(CONV_W) if j != 15]:
                        for cc_ in cs:
                            uv = ub[cc_ % 2][:].rearrange("p (r t) -> p r t", t=GW)
                            yv = ybuf[:, cc_, :].rearrange("p (r t) -> p r t", t=GW)
                            if j == 15:
                                V(lambda cc_=cc_: nc.vector.tensor_scalar(out=ybuf[:, cc_, :], in0=ub[cc_ % 2][:], scalar1=wdw[:, cc_, 15:16],
                                                                          scalar2=cv[:, cc_, 0:1], op0=ALU.mult, op1=ALU.add),
                                  r=[("ub", cc_ % 2), "wdw", "cv"], w=[("y", cc_)])
                            else:
                                d_ = j - 15
                                lo_o, hi_o = max(0, -d_), GW - max(0, d_)
                                lo_i, hi_i = max(0, d_), GW - max(0, -d_)
                                V(lambda cc_=cc_, j=j, uv=uv, yv=yv, lo_o=lo_o, hi_o=hi_o, lo_i=lo_i, hi_i=hi_i: nc.vector.scalar_tensor_tensor(
                                    out=yv[:, :, lo_o:hi_o], in0=uv[:, :, lo_i:hi_i], scalar=wdw[:, cc_, j:j + 1], in1=yv[:, :, lo_o:hi_o],
                                    op0=ALU.mult, op1=ALU.add), r=[("ub", cc_ % 2), ("y", cc_)], w=[("y", cc_)])
                    for cc_ in cs:
                        A(lambda cc_=cc_: nc.scalar.activation(out=ysq[cc_ % 2][:], in_=ybuf[:, cc_, :], func=AF.Square), r=[("y", cc_)], w=[("ysq", cc_ % 2)])
                        for hf in range(NH2):
                            P(lambda cc_=cc_, hf=hf: nc.tensor.matmul(out=ps[:, hf, 0:NTB], lhsT=CM("ones"), rhs=ybuf[:, cc_, hf * NTB:(hf + 1) * NTB],
                                                                     start=(cc_ == 0), stop=(cc_ == NCC - 1)), r=[("y", cc_)], w=[("s1", hf)])
                            P(lambda cc_=cc_, hf=hf: nc.tensor.matmul(out=ps[:, 2 + hf, 0:NTB], lhsT=CM("ones"), rhs=ysq[cc_ % 2][:, hf * NTB:(hf + 1) * NTB],
                                                                     start=(cc_ == 0), stop=(cc_ == NCC - 1)), r=[("ysq", cc_ % 2)], w=[("s2", hf)])
                for hf in range(NH2):
                    sl = slice(hf * NTB, (hf + 1) * NTB)
                    A(lambda hf=hf, sl=sl: nc.scalar.mul(out=mu[:, sl], in_=ps[:, hf, 0:NTB], mul=1.0 / DC), r=[("s1", hf)], w=[("mu", hf)])
                    V(lambda hf=hf, sl=sl: nc.vector.tensor_tensor(out=rsv[:, sl], in0=mu[:, sl], in1=mu[:, sl], op=ALU.mult), r=[("mu", hf)], w=[("rsv", hf)])
                    V(lambda hf=hf, sl=sl: nc.vector.scalar_tensor_tensor(out=rsv[:, sl], in0=ps[:, 2 + hf, 0:NTB], scalar=1.0 / DC, in1=rsv[:, sl],
                                                                          op0=ALU.mult, op1=ALU.subtract), r=[("s2", hf), ("rsv", hf)], w=[("rsv", hf)])
                    A(lambda hf=hf, sl=sl: nc.scalar.activation(out=rsv[:, sl], in_=rsv[:, sl], func=AF.Sqrt, bias=EPS, scale=1.0), r=[("rsv", hf)], w=[("rsv", hf)])
                    V(lambda hf=hf, sl=sl: nc.vector.reciprocal(out=rsv[:, sl], in_=rsv[:, sl]), r=[("rsv", hf)], w=[("rsv", hf)])
                rk = [("rsv", hf) for hf in range(NH2)]; mk = [("mu", hf) for hf in range(NH2)]
                for cc_ in range(NCC):
                    i_ = cc_ % 2
                    G(lambda cc_=cc_, i_=i_: nc.gpsimd.tensor_tensor(out=t1[i_][:], in0=ybuf[:, cc_, :], in1=mu[:], op=ALU.subtract), r=[("y", cc_)] + mk, w=[("t1", i_)])
                    V(lambda i_=i_: nc.vector.tensor_tensor(out=t1[i_][:], in0=t1[i_][:], in1=rsv[:], op=ALU.mult), r=[("t1", i_)] + rk, w=[("t1", i_)])
                    A(lambda cc_=cc_, i_=i_: nc.scalar.activation(out=actT[:, c.NH + cc_, :], in_=t1[i_][:], func=AF.Silu, scale=cv[:, cc_, 1:2], bias=cv[:, cc_, 2:3]),
                      r=[("t1", i_), "cv"], w=[("actT", c.NH + cc_)])
                S.flush()
                if stop == "B4":
                    return nc

            with contextlib.ExitStack() as ph:
                T = lambda n, s, d=F32: ph.enter_context(nc.sbuf_tensor(uname(n), list(s), d))
                ps = ph.enter_context(nc.psum_tensor("psO", [128, 8, 512], F32))
                Tw = {"st": [T("stO%d" % i, [128, KP, 256]) for i in range(3)], "wb": [T("wbO%d" % i, [128, KD, 256], BF16) for i in range(2)],
                      "wi": [0], "si": [0]}
                gam = T("gam", [128, D])
                xs_ = [T("xsl%d" % i, [128, 256]) for i in range(3)]; tm_ = [T("tml%d" % i, [128, 256]) for i in range(3)]
                DM(lambda: nc.sync.dma_start(out=gam[:], in_=mod_d[0, 2 * D:3 * D].partition_broadcast(128)), w=["gam"])
                n_ = 0
                for c0 in range(0, D, 256):
                    wb, wkey = load_cast_w(Tw, lambda k0, k1, c0=c0: w_out[k0 * 128:k1 * 128, c0:c0 + 256], KD, 256, "O")
                    for t in range(NT):
                        bank = n_ % 6; i_ = n_ % 3; n_ += 1
                        rows = slice(t * 128, (t + 1) * 128)
                        DM(lambda i_=i_, rows=rows, c0=c0: nc.sync.dma_start(out=xs_[i_][:], in_=x_own[rows, c0:c0 + 256]), w=[("xsl", i_)])
                        for k in range(KD):
                            P(lambda k=k, rows=rows, wb=wb, bank=bank: nc.tensor.matmul(out=ps[:, bank, 0:256], lhsT=actT[:, k, rows], rhs=wb[:, k, :],
                                                                                       start=(k == 0), stop=(k == KD - 1)), r=[wkey], w=[("pb", bank)])
                        V(lambda i_=i_, bank=bank, c0=c0: nc.vector.tensor_tensor(out=tm_[i_][:], in0=ps[:, bank, 0:256], in1=gam[:, c0:c0 + 256], op=ALU.mult),
                          r=[("pb", bank), "gam"], w=[("tml", i_)])
                        G(lambda i_=i_: nc.gpsimd.tensor_tensor(out=tm_[i_][:], in0=tm_[i_][:], in1=xs_[i_][:], op=ALU.add),
                          r=[("tml", i_), ("xsl", i_)], w=[("tml", i_)])
                        DM(lambda i_=i_, rows=rows, c0=c0: nc.sync.dma_start(out=xlat_d[rows, c0:c0 + 256], in_=tm_[i_][:]), r=[("tml", i_)], w=["xlat_d"])
                S.flush()
                if stop == "B5":
                    return nc

        gla_scope.close()
        moe = contextlib.ExitStack()
        with moe:
            MT = lambda n, s, d=F32: moe.enter_context(nc.sbuf_tensor(uname(n), list(s), d))
            Wr_all = MT("Wr_all", [128, NT, E]); mask_all = MT("mask_all", [128, NT, E])
            idxg = MT("idxg", [128, NB * E], I32); idxs = MT("idxs", [128, NB * E], I32); wsl = MT("wsl", [128, NB * E])
            with contextlib.ExitStack() as shs:
                h2T = shs.enter_context(nc.sbuf_tensor("s_h2T", [128, KD, TOK], BF16))
                with contextlib.ExitStack() as ph:
                    T = lambda n, s, d=F32: ph.enter_context(nc.sbuf_tensor(uname(n), list(s), d))
                    ps = ph.enter_context(nc.psum_tensor("psR", [128, 8, 512], F32))
                    Tn = norm_bufs(T, with_xb=True)
                    h2f = T("h2f", [128, KD, 128]); wrt = T("wrt", [128, KD, E]); brb = T("brb", [128, E])
                    sc = [T("sc%d" % i, [128, E]) for i in range(2)]; sel = [T("sel%d" % i, [128, E]) for i in range(2)]
                    sel2 = [T("sel2%d" % i, [128, E]) for i in range(2)]
                    sm = [T("sm%d" % i, [128, 64]) for i in range(2)]
                    DM(lambda: nc.sync.dma_start(out=wrt[:], in_=w_rt.rearrange("(k p) n -> p k n", p=128)), w=["wrt"])
                    DM(lambda: nc.sync.dma_start(out=brb[:], in_=b_rt.partition_broadcast(128)), w=["brb"])
                    lvl6 = int(os.environ.get('KDBG_B6', '999')); rc = [0]

                    def VR(fn, r=(), w=()):
                        rc[0] += 1
                        if rc[0] <= lvl6: V(fn, r, w)

                    def AR(fn, r=(), w=()):
                        rc[0] += 1
                        if rc[0] <= lvl6: A(fn, r, w)

                    for t in range(NT):
                        rc[0] = 0
                        rows = slice(t * 128, (t + 1) * 128); i_ = t % 2
                        n6 = int(os.environ.get('KDBG_B6N', '9'))
                        norm_tile(Tn, ps, xlat_d[rows, :], Gf, shf, lambda k, rows=rows: (h2T[:, k, rows], ("h2T", k)), t,
                                  xs2_dst=(xs2_d[rows, :] if n6 >= 1 else None), h2f=((lambda k: (h2f[:, k, :], ("h2f", k))) if n6 >= 2 else None), tpbanks=(4, 6))
                        for k in range(KD if n6 >= 3 else 0):
                            P(lambda k=k: nc.tensor.matmul(out=ps[:, 5, 0:E], lhsT=h2f[:, k, :], rhs=wrt[:, k, :], start=(k == 0), stop=(k == KD - 1)),
                              r=[("h2f", k), "wrt"], w=[("bk", 5)])
                        m = sm[i_]
                        AR(lambda i_=i_: nc.scalar.activation(out=sc[i_][:], in_=ps[:, 5, 0:E], func=AF.Sigmoid), r=[("bk", 5)], w=[("sc", i_)])
                        VR(lambda i_=i_: nc.vector.tensor_tensor(out=sel[i_][:], in0=sc[i_][:], in1=brb[:], op=ALU.add), r=[("sc", i_), "brb"], w=[("sel", i_)])
                        k1 = ("rt", i_)
                        VR(lambda i_=i_, m=m: nc.vector.tensor_reduce(out=m[:, 0:8], in_=sel[i_][:].rearrange("p (g e) -> p g e", e=c.EPG), axis=AX.X, op=ALU.max),
                          r=[("sel", i_)], w=[k1])
                        for g in range(NGRP):
                            gs_ = slice(g * c.EPG, (g + 1) * c.EPG)
                            VR(lambda i_=i_, m=m, g=g, gs_=gs_: nc.vector.tensor_scalar(out=sel2[i_][:, gs_], in0=sel[i_][:, gs_], scalar1=m[:, g:g + 1], scalar2=-1e9,
                                                                                       op0=ALU.is_equal, op1=ALU.mult), r=[k1, ("sel", i_)], w=[("sel2", i_)])
                        VR(lambda i_=i_: nc.vector.tensor_tensor(out=sel2[i_][:], in0=sel2[i_][:], in1=sel[i_][:], op=ALU.add), r=[("sel2", i_), ("sel", i_)], w=[("sel2", i_)])
                        VR(lambda i_=i_, m=m: nc.vector.tensor_reduce(out=m[:, 8:16], in_=sel2[i_][:].rearrange("p (g e) -> p g e", e=c.EPG), axis=AX.X, op=ALU.max),
                          r=[("sel2", i_)], w=[k1])
                        VR(lambda m=m: nc.vector.tensor_tensor(out=m[:, 8:16], in0=m[:, 8:16], in1=m[:, 0:8], op=ALU.add), r=[k1], w=[k1])
                        VR(lambda m=m: nc.vector.max(out=m[:, 16:24], in_=m[:, 8:16]), r=[k1], w=[k1])
                        VR(lambda m=m: nc.vector.tensor_scalar(out=m[:, 24:32], in0=m[:, 8:16], scalar1=m[:, 16 + TOPG - 1:16 + TOPG], scalar2=None, op0=ALU.is_ge), r=[k1], w=[k1])
                        VR(lambda m=m: nc.vector.tensor_scalar(out=m[:, 32:40], in0=m[:, 24:32], scalar1=-1.0, scalar2=1e9, op0=ALU.add, op1=ALU.mult), r=[k1], w=[k1])
                        for g in range(NGRP):
                            gs_ = slice(g * c.EPG, (g + 1) * c.EPG)
                            VR(lambda i_=i_, m=m, g=g, gs_=gs_: nc.vector.tensor_scalar(out=sel2[i_][:, gs_], in0=sel[i_][:, gs_], scalar1=m[:, 32 + g:33 + g], scalar2=None,
                                                                                       op0=ALU.add), r=[k1, ("sel", i_), ("sel2", i_)], w=[("sel2", i_)])
                        VR(lambda i_=i_, m=m: nc.vector.max(out=m[:, 40:48], in_=sel2[i_][:]), r=[("sel2", i_), k1], w=[k1])
                        VR(lambda i_=i_, m=m, t=t: nc.vector.tensor_scalar(out=mask_all[:, t, :], in0=sel2[i_][:], scalar1=m[:, 40 + TOPK - 1:40 + TOPK], scalar2=None, op0=ALU.is_ge),
                          r=[("sel2", i_), k1], w=[("mask", t)])
                        VR(lambda i_=i_, t=t: nc.vector.tensor_tensor(out=sel[i_][:], in0=sc[i_][:], in1=mask_all[:, t, :], op=ALU.mult), r=[("sc", i_), ("mask", t), ("sel", i_), ("sel2", i_)], w=[("sel", i_)])
                        VR(lambda i_=i_, m=m: nc.vector.tensor_reduce(out=m[:, 48:49], in_=sel[i_][:], axis=AX.X, op=ALU.add), r=[("sel", i_), k1], w=[k1])
                        VR(lambda m=m: nc.vector.reciprocal(out=m[:, 49:50], in_=m[:, 48:49]), r=[k1], w=[k1])
                        VR(lambda i_=i_, m=m, t=t: nc.vector.tensor_scalar(out=Wr_all[:, t, :], in0=sel[i_][:], scalar1=m[:, 49:50], scalar2=RSCALE, op0=ALU.mult, op1=ALU.mult),
                          r=[("sel", i_), k1], w=[("Wr", t)])
                    S.flush()
                    if stop == "B6":
                        return nc

                with contextlib.ExitStack() as ph:
                    T = lambda n, s, d=F32: ph.enter_context(nc.sbuf_tensor(uname(n), list(s), d))
                    ps = ph.enter_context(nc.psum_tensor("psI", [128, 8, 512], F32))
                    pos_all = T("pos_all", [128, NT, E]); vals = T("vals", [128, NT, E, 2])
                    oh = [T("oh%d" % i, [128, C]) for i in range(4)]
                    tab = T("tab", [128, NB, 2 * E]); eq0 = T("eq0", [128, NB, E]); tf = T("tf", [128, NB, E]); tg = T("tg", [128, NB, E])
                    for j in range(NT):
                        for i in range(j + 1):
                            P(lambda i=i, j=j: nc.tensor.matmul(out=ps[:, 4 + j % 2, 0:E], lhsT=(CM("ustr") if i == j else CM("ones")), rhs=mask_all[:, i, :],
                                                               start=(i == 0), stop=(i == j)), w=[("pp", j % 2)])
                        V(lambda j=j: nc.vector.tensor_copy(out=pos_all[:, j, :], in_=ps[:, 4 + j % 2, 0:E]), r=[("pp", j % 2)], w=["pos"])
                    for i in range(NT):
                        V(lambda i=i: nc.vector.tensor_scalar(out=vals[:, i, :, 0], in0=mask_all[:, i, :], scalar1=0.0, scalar2=CM("tokid1")[:, i:i + 1],
                                                              op0=ALU.mult, op1=ALU.add), w=["vals"])
                        G(lambda i=i: nc.gpsimd.tensor_copy(out=vals[:, i, :, 1], in_=Wr_all[:, i, :]), w=["vals"])
                    n_ = 0
                    for e in range(E):
                        for i in range(NT):
                            o_ = n_ % 4; n_ += 1
                            V(lambda e=e, i=i, o_=o_: nc.vector.tensor_scalar(out=oh[o_][:], in0=CM("iotaC"), scalar1=pos_all[:, i, e:e + 1], scalar2=mask_all[:, i, e:e + 1],
                                                                              op0=ALU.is_equal, op1=ALU.mult), r=["pos"], w=[("oh", o_)])
                            for b in range(NB):
                                P(lambda e=e, i=i, o_=o_, b=b: nc.tensor.matmul(out=ps[:, b, 2 * e:2 * e + 2], lhsT=oh[o_][:, b * 128:(b + 1) * 128], rhs=vals[:, i, e, :],
                                                                               start=(i == 0), stop=(i == NT - 1)), r=[("oh", o_), "vals"], w=[("ib", b)])
                    for b in range(NB):
                        V(lambda b=b: nc.vector.tensor_copy(out=tab[:, b, :], in_=ps[:, b, 0:2 * E]), r=[("ib", b)], w=["tab"])
                    tv = tab[:].rearrange("p b (e two) -> p b e two", two=2)
                    fl = lambda t_: t_[:].rearrange("p b e -> p (b e)")
                    V(lambda: nc.vector.tensor_copy(out=wsl[:], in_=tv[:, :, :, 1].rearrange("p b e -> p (b e)")), r=["tab"], w=["wsl"])
                    V(lambda: nc.vector.tensor_scalar(out=eq0[:], in0=tv[:, :, :, 0], scalar1=0.0, scalar2=None, op0=ALU.is_equal), r=["tab"], w=["eq0"])
                    V(lambda: nc.vector.scalar_tensor_tensor(out=tf[:], in0=eq0[:], scalar=float(TOK + 1), in1=tv[:, :, :, 0], op0=ALU.mult, op1=ALU.add), r=["eq0", "tab"], w=["tf"])
                    V(lambda: nc.vector.tensor_scalar(out=tf[:], in0=tf[:], scalar1=-1.0, scalar2=None, op0=ALU.add), r=["tf"], w=["tf"])
                    V(lambda: nc.vector.tensor_copy(out=idxg[:], in_=fl(tf)), r=["tf"], w=["idxg"])
                    V(lambda: nc.vector.tensor_scalar(out=tg[:], in0=eq0[:], scalar1=CM("trash")[:, 0:1], scalar2=None, op0=ALU.mult), r=["eq0"], w=["tg"])
                    V(lambda: nc.vector.tensor_tensor(out=tg[:], in0=tg[:], in1=eq0[:], op=ALU.add), r=["tg", "eq0"], w=["tg"])
                    V(lambda: nc.vector.tensor_tensor(out=tg[:], in0=tg[:], in1=tv[:, :, :, 0], op=ALU.add), r=["tg", "tab"], w=["tg"])
                    V(lambda: nc.vector.tensor_scalar(out=tg[:], in0=tg[:], scalar1=-1.0, scalar2=None, op0=ALU.add), r=["tg"], w=["tg"])
                    V(lambda: nc.vector.tensor_copy(out=idxs[:], in_=fl(tg)), r=["tg"], w=["idxs"])
                    if dbg:
                        d1 = nc.dram_tensor("dbg_idxg", [128, NB * E], I32, kind="ExternalOutput").ap()
                        d2 = nc.dram_tensor("dbg_idxs", [128, NB * E], I32, kind="ExternalOutput").ap()
                        d3 = nc.dram_tensor("dbg_wsl", [128, NB * E], F32, kind="ExternalOutput").ap()
                        d4 = nc.dram_tensor("dbg_wr", [128, NT * E], F32, kind="ExternalOutput").ap()
                        d5 = nc.dram_tensor("dbg_pos", [128, NT * E], F32, kind="ExternalOutput").ap()
                        DM(lambda: nc.sync.dma_start(out=d1, in_=idxg[:]), r=["idxg"])
                        DM(lambda: nc.sync.dma_start(out=d2, in_=idxs[:]), r=["idxs"])
                        DM(lambda: nc.sync.dma_start(out=d3, in_=wsl[:]), r=["wsl"])
                        DM(lambda: nc.sync.dma_start(out=d4, in_=Wr_all[:].rearrange("p t e -> p (t e)")))
                        DM(lambda: nc.sync.dma_start(out=d5, in_=pos_all[:].rearrange("p t e -> p (t e)")), r=["pos"])
                    S.flush()
                    if stop == "B7":
                        return nc

                with contextlib.ExitStack() as ph:
                    T = lambda n, s, d=F32: ph.enter_context(nc.sbuf_tensor(uname(n), list(s), d))
                    ps = ph.enter_context(nc.psum_tensor("psS", [128, 8, 512], F32))
                    Tw = {"st": [T("stS%d" % i, [128, KP, 256]) for i in range(3)], "wb": [T("wbS%d" % i, [128, KD, 256], BF16) for i in range(3)],
                          "wi": [0], "si": [0]}
                    HsT = T("HsT", [128, c.NSB, TOK], BF16); tsl = [T("tsl%d" % i, [128, NTB]) for i in range(2)]
                    ysb = [T("ysb%d" % i, [128, 256]) for i in range(3)]; zt = T("zt", [128, c.DQ])
                    V(lambda: nc.vector.memset(zt[:], 0.0), w=["zt"])
                    for q in range(c.NQ):
                        DM(lambda q=q: nc.sync.dma_start(out=y_q[q][TOK:TOK + 128, :], in_=zt[:]), r=["zt"], w=[("yq", q)])
                    n_ = 0
                    for hb in range(c.NSB):
                        wg_, kg = load_cast_w(Tw, lambda k0, k1, hb=hb: w_sg[k0 * 128:k1 * 128, hb * 128:(hb + 1) * 128], KD, 128, "S")
                        wu_, ku = load_cast_w(Tw, lambda k0, k1, hb=hb: w_su[k0 * 128:k1 * 128, hb * 128:(hb + 1) * 128], KD, 128, "S")
                        for tb in range(TOK // NTB):
                            bg_ = (2 * n_) % 6; bu_ = (2 * n_ + 1) % 6; i_ = n_ % 2; n_ += 1
                            for (wb_, wk_, bank) in ((wg_, kg, bg_), (wu_, ku, bu_)):
                                for k in range(KD):
                                    P(lambda k=k, wb_=wb_, bank=bank, tb=tb: nc.tensor.matmul(out=ps[:, bank, 0:NTB], lhsT=wb_[:, k, 0:128], rhs=h2T[:, k, tb * NTB:(tb + 1) * NTB],
                                                                                             start=(k == 0), stop=(k == KD - 1)), r=[wk_], w=[("pb", bank)])
                            A(lambda i_=i_, bg_=bg_: nc.scalar.activation(out=tsl[i_][:], in_=ps[:, bg_, 0:NTB], func=AF.Silu), r=[("pb", bg_)], w=[("tsl", i_)])
                            V(lambda i_=i_, bu_=bu_, hb=hb, tb=tb: nc.vector.tensor_tensor(out=HsT[:, hb, tb * NTB:(tb + 1) * NTB], in0=tsl[i_][:], in1=ps[:, bu_, 0:NTB], op=ALU.mult),
                              r=[("tsl", i_), ("pb", bu_)], w=["HsT"])
                    n_ = 0
                    for c0 in range(0, D, 256):
                        wb, wkey = load_cast_w(Tw, lambda k0, k1, c0=c0: w_sd[k0 * 128:k1 * 128, c0:c0 + 256], c.NSB, 256, "S")
                        q = c0 // c.DQ; qo = c0 % c.DQ
                        for t in range(NT):
                            bank = n_ % 6; i_ = n_ % 3; n_ += 1
                            rows = slice(t * 128, (t + 1) * 128)
                            for kk in range(c.NSB):
                                P(lambda kk=kk, wb=wb, bank=bank, rows=rows: nc.tensor.matmul(out=ps[:, bank, 0:256], lhsT=HsT[:, kk, rows], rhs=wb[:, kk, :],
                                                                                             start=(kk == 0), stop=(kk == c.NSB - 1)), r=[wkey, "HsT"], w=[("pb", bank)])
                            VA(lambda i_=i_, bank=bank: nc.vector.tensor_copy(out=ysb[i_][:], in_=ps[:, bank, 0:256]),
                               lambda i_=i_, bank=bank: nc.scalar.copy(out=ysb[i_][:], in_=ps[:, bank, 0:256]), r=[("pb", bank)], w=[("ysb", i_)])
                            DM(lambda i_=i_, q=q, qo=qo, rows=rows: nc.sync.dma_start(out=y_q[q][rows, qo:qo + 256], in_=ysb[i_][:]), r=[("ysb", i_)], w=[("yq", q)])
                    S.flush()
                    if stop == "B8":
                        return nc

            with contextlib.ExitStack() as ph:
                T = lambda n, s, d=F32: ph.enter_context(nc.sbuf_tensor(uname(n), list(s), d))
                ps = ph.enter_context(nc.psum_tensor("psE", [128, 6, 512], F32))
                psT = ph.enter_context(nc.psum_tensor("psT", [128, 2, 1024], BF16))
                NXE = NB + (1 if C < 512 else 0)
                Xe = [T("Xe%d" % i, [128, D], BF16) for i in range(NXE)]
                XeT = T("XeT", [128, KD, C], BF16)
                HT = [T("HT%d" % i, [128, c.NHB, C], BF16) for i in range(2)]
                KH = max(1, KD // 2)
                stE = [T("stE%d" % i, [128, KH, 128]) for i in range(4)]
                wgb = [T("wgb%d" % i, [128, KD, 128], BF16) for i in range(3)]
                stD = [T("stD%d" % i, [128, c.NHB, c.DW]) for i in range(2)]
                wdb = [T("wdb%d" % i, [128, c.NHB, c.DW], BF16) for i in range(2)]
                Ost = [T("Ost%d" % i, [128, c.DQ]) for i in range(NB)]
                tsl = [T("tse%d" % i, [128, C]) for i in range(2)]
                for i in range(NXE):
                    V(lambda i=i: nc.vector.memset(Xe[i][:], 0.0), w=[("Xe", i)])
                sn = 0; gn = 0; dn = 0; on = 0; bn = 0; tn = 0
                xn = [0]
                reg_g = nc.gpsimd.to_reg(TOK - 1); reg_s = nc.gpsimd.to_reg(TOK + 127)

                def issue_gathers(e):
                    xr_ = []
                    for b in range(NB):
                        r_ = xn[0] % NXE; xn[0] += 1; xr_.append(r_)
                        col = b * E + e
                        GD(lambda r_=r_, col=col: nc.gpsimd.indirect_dma_start(
                            out=Xe[r_][:], out_offset=None, in_=xs2_d, in_offset=bass.IndirectOffsetOnAxis(ap=idxg[:, col:col + 1], axis=0),
                            bounds_check=reg_g, oob_is_err=False), w=[("Xe", r_)])
                    return xr_

                xr_next = issue_gathers(0)
                for e in range(E):
                    xr = xr_next
                    for k in range(KD):
                        tb_ = tn % 2; tn += 1
                        for b in range(NB):
                            P(lambda k=k, b=b, tb_=tb_, r_=xr[b]: nc.tensor.transpose(out=psT[:, tb_, b * 128:(b + 1) * 128], in_=Xe[r_][:, k * 128:(k + 1) * 128], identity=idb[:]),
                              r=[("Xe", xr[b])], w=[("pT", tb_)])
                        VA(lambda k=k, tb_=tb_: nc.vector.tensor_scalar(out=XeT[:, k, :], in0=psT[:, tb_, 0:C], scalar1=Gf[:, k:k + 1], scalar2=shf[:, k:k + 1], op0=ALU.mult, op1=ALU.add),
                           lambda k=k, tb_=tb_: nc.scalar.activation(out=XeT[:, k, :], in_=psT[:, tb_, 0:C], func=AF.Identity, scale=Gf[:, k:k + 1], bias=shf[:, k:k + 1]),
                           r=[("pT", tb_)], w=[("XeT", k)])
                    if e + 1 < E:
                        xr_next = issue_gathers(e + 1)
                    hbuf = HT[e % 2]
                    for hb in range(c.NHB):
                        banks = []
                        for wsrc in (w_eg, w_eu):
                            g_ = gn % 3; gn += 1
                            base = (e * c.NHB + hb) * 128
                            for k0 in range(0, KD, KH):
                                s_ = sn % 4; sn += 1
                                DM(lambda wsrc=wsrc, base=base, k0=k0, s_=s_: nc.sync.dma_start(
                                    out=stE[s_][:], in_=wsrc[base:base + 128, k0 * 128:(k0 + KH) * 128].rearrange("p (k n) -> p k n", n=128)), w=[("stE", s_)])
                                V(lambda g_=g_, k0=k0, s_=s_: nc.vector.tensor_copy(out=wgb[g_][:, k0:k0 + KH, :], in_=stE[s_][:]), r=[("stE", s_)], w=[("wgb", g_)])
                            bank = bn % 4; bn += 1; banks.append(bank)
                            for k in range(KD):
                                P(lambda k=k, g_=g_, bank=bank: nc.tensor.matmul(out=ps[:, bank, 0:C], lhsT=wgb[g_][:, k, :], rhs=XeT[:, k, :], start=(k == 0), stop=(k == KD - 1)),
                                  r=[("wgb", g_), ("XeT", k)], w=[("pb", bank)])
                        i_ = (e * c.NHB + hb) % 2
                        A(lambda i_=i_, bank=banks[0]: nc.scalar.activation(out=tsl[i_][:], in_=ps[:, bank, 0:C], func=AF.Silu), r=[("pb", banks[0])], w=[("tse", i_)])
                        V(lambda i_=i_, hb=hb, hbuf=hbuf, bank=banks[1]: nc.vector.tensor_tensor(out=hbuf[:, hb, :], in0=tsl[i_][:], in1=ps[:, bank, 0:C], op=ALU.mult),
                          r=[("tse", i_), ("pb", banks[1])], w=[("HT", e % 2)])
                    DW = c.DW
                    for q in range(c.NQ):
                        for sub in range(c.DQ // DW):
                            d_ = dn % 2; dn += 1
                            c0 = q * c.DQ + sub * DW
                            DM(lambda e=e, c0=c0, d_=d_: nc.sync.dma_start(out=stD[d_][:], in_=w_ed[e * c.DE:(e + 1) * c.DE, c0:c0 + DW].rearrange("(k p) n -> p k n", p=128)),
                               w=[("stD", d_)])
                            VA(lambda d_=d_: nc.vector.tensor_copy(out=wdb[d_][:], in_=stD[d_][:]),
                               lambda d_=d_: nc.scalar.copy(out=wdb[d_][:], in_=stD[d_][:]), r=[("stD", d_)], w=[("wdb", d_)])
                            for b in range(NB):
                                bank = 4 + (on % 2); on += 1
                                col = b * E + e
                                for kk in range(c.NHB):
                                    P(lambda kk=kk, b=b, d_=d_, bank=bank, hbuf=hbuf: nc.tensor.matmul(out=ps[:, bank, 0:DW], lhsT=hbuf[:, kk, b * 128:(b + 1) * 128], rhs=wdb[d_][:, kk, :],
                                                                                                  start=(kk == 0), stop=(kk == c.NHB - 1)), r=[("wdb", d_), ("HT", e % 2)], w=[("pd", bank)])
                                osl = slice(sub * DW, (sub + 1) * DW)
                                VA(lambda b=b, bank=bank, col=col, osl=osl: nc.vector.tensor_scalar(out=Ost[b][:, osl], in0=ps[:, bank, 0:DW], scalar1=wsl[:, col:col + 1], scalar2=None, op0=ALU.mult),
                                   lambda b=b, bank=bank, col=col, osl=osl: nc.scalar.activation(out=Ost[b][:, osl], in_=ps[:, bank, 0:DW], func=AF.Copy, scale=wsl[:, col:col + 1]),
                                   r=[("pd", bank)], w=[("Ost", b)])
                        for b in range(NB):
                            col = b * E + e
                            GD(lambda b=b, q=q, col=col: nc.gpsimd.indirect_dma_start(
                                out=y_q[q], out_offset=bass.IndirectOffsetOnAxis(ap=idxs[:, col:col + 1], axis=0), in_=Ost[b][:], in_offset=None,
                                bounds_check=reg_s, oob_is_err=True, compute_op=ALU.add), r=[("Ost", b)], w=[("yq", q)])
                S.flush()
                if stop == "B9":
                    return nc

            with contextlib.ExitStack() as ph:
                T = lambda n, s, d=F32: ph.enter_context(nc.sbuf_tensor(uname(n), list(s), d))
                xl = [T("xl%d" % i, [128, D]) for i in range(2)]; yt = [T("yt%d" % i, [128, D]) for i in range(2)]
                gaf = T("gaf", [128, D]); gfin = T("gfin", [128, D]); junk = T("junkF", [128, D], BF16)
                ss = T("ssF", [128, 2]); rs = T("rsF", [128, 2])
                DM(lambda: nc.sync.dma_start(out=gaf[:], in_=mod_d[0, 5 * D:6 * D].partition_broadcast(128)), w=["gaf"])
                DM(lambda: nc.sync.dma_start(out=gfin[:], in_=gvecs[2].partition_broadcast(128)), w=["gfin"])
                for t in range(NT):
                    i_ = t % 2; rows = slice(t * 128, (t + 1) * 128)
                    DM(lambda i_=i_, rows=rows: nc.sync.dma_start(out=xl[i_][:], in_=xlat_d[rows, :]), w=[("xl", i_)])
                    for q in range(c.NQ):
                        DM(lambda i_=i_, rows=rows, q=q: nc.sync.dma_start(out=yt[i_][:, q * c.DQ:(q + 1) * c.DQ], in_=y_q[q][rows, :]), w=[("yt", i_)])
                    V(lambda i_=i_: nc.vector.tensor_tensor(out=yt[i_][:], in0=yt[i_][:], in1=gaf[:], op=ALU.mult), r=[("yt", i_), "gaf"], w=[("yt", i_)])
                    G(lambda i_=i_: nc.gpsimd.tensor_tensor(out=yt[i_][:], in0=yt[i_][:], in1=xl[i_][:], op=ALU.add), r=[("yt", i_), ("xl", i_)], w=[("yt", i_)])
                    G(lambda i_=i_: nc.gpsimd.memset(ss[:, i_:i_ + 1], 0.0), w=[("ssF", i_)])
                    A(lambda i_=i_: nc.scalar.activation(out=junk[:], in_=yt[i_][:], func=AF.Square, scale=float(D) ** -0.5, accum_out=ss[:, i_:i_ + 1]),
                      r=[("yt", i_), ("ssF", i_)], w=[("ssF", i_), "junkF"])
                    A(lambda i_=i_: nc.scalar.activation(out=rs[:, i_:i_ + 1], in_=ss[:, i_:i_ + 1], func=AF.Sqrt, bias=EPS, scale=1.0), r=[("ssF", i_)], w=[("rsF", i_)])
                    V(lambda i_=i_: nc.vector.reciprocal(out=rs[:, i_:i_ + 1], in_=rs[:, i_:i_ + 1]), r=[("rsF", i_)], w=[("rsF", i_)])
                    V(lambda i_=i_: nc.vector.scalar_tensor_tensor(out=xl[i_][:], in0=yt[i_][:], scalar=rs[:, i_:i_ + 1], in1=gfin[:], op0=ALU.mult, op1=ALU.mult),
                      r=[("yt", i_), ("rsF", i_), "gfin", ("xl", i_)], w=[("xl", i_)])
                    DM(lambda i_=i_, rows=rows: nc.sync.dma_start(out=out[rows, :], in_=xl[i_][:]), r=[("xl", i_)], w=["out"])
                S.flush()
    return nc


def _prep_inputs(c, inp):
    f = lambda a: np.ascontiguousarray(a, dtype=np.float32)
    D, TOK, KD = c.D, c.TOK, c.KD
    x = f(inp["x"])[0]; ctx = f(inp["ctx"])[0]
    w_in = f(inp["w_in"])[0]
    DK, DG, DC = c.DK, c.DG, c.DC
    oQ, oK, oV, oR = 0, DK, 2 * DK, 2 * DK + DG
    oGF = oR + DG; oGB = oGF + 16; oCA = oGB + 16; oCB = oCA + DC
    wa = w_in[:, oCA:oCA + DC].reshape(D, c.NCC, 128); wb_ = w_in[:, oCB:oCB + DC].reshape(D, c.NCC, 128)
    w_ab = np.ascontiguousarray(np.concatenate([wa, wb_], axis=2).reshape(D, 2 * DC))
    wgu = f(inp["w_gate_up"])[0]; bgu = f(inp["b_gate_up"])[0]
    wgf, wgb = w_in[:, oGF:oGF + 16], w_in[:, oGB:oGB + 16]
    weg = f(inp["w_e_gate"])[0]; weu = f(inp["w_e_up"])[0]

    def relay(w):
        return np.ascontiguousarray(w.reshape(c.E, KD, 128, c.NHB, 128).transpose(0, 3, 2, 1, 4).reshape(c.E * c.NHB * 128, KD * 128))
    common = {
        "ctx2": np.ascontiguousarray(np.concatenate([ctx, ctx[::-1]], 0)),
        "cc": np.ascontiguousarray(np.stack([f(inp["c"])[0], f(inp["c_ctx"])], 0)),
        "w_ada": f(inp["w_ada"])[0], "b_ada": f(inp["b_ada"])[0],
        "gvecs": np.ascontiguousarray(np.stack([f(inp["g_norm_mix"])[0], f(inp["g_norm_ffn"])[0], f(inp["g_final"])], 0)),
        "w_q": np.ascontiguousarray(w_in[:, oQ:oQ + DK]), "w_k": np.ascontiguousarray(w_in[:, oK:oK + DK]),
        "w_v": np.ascontiguousarray(w_in[:, oV:oV + DG]), "w_r": np.ascontiguousarray(w_in[:, oR:oR + DG]),
        "w_ab": w_ab, "w_g": np.ascontiguousarray(w_in[:, oGF:oGF + 32]),
        "wgu": np.ascontiguousarray(wgu.reshape(32, DK)), "bgu": bgu,
        "g_gla": f(inp["g_gla_out"])[0], "w_dw": f(inp["w_dw"])[0],
        "cvec": np.ascontiguousarray(np.stack([f(inp["b_dw"])[0], f(inp["g_conv_ln"])[0], f(inp["b_conv_ln"])[0]], 0)),
        "w_out": f(inp["w_out"])[0], "w_rt": f(inp["w_router"])[0], "b_rt": f(inp["b_router"])[0],
        "w_eg": relay(weg), "w_eu": relay(weu), "w_ed": np.ascontiguousarray(f(inp["w_e_down"])[0].reshape(c.E * c.DE, D)),
        "w_sg": f(inp["w_s_gate"])[0], "w_su": f(inp["w_s_up"])[0], "w_sd": f(inp["w_s_down"])[0],
    }
    maps = []
    for i in range(c.NCORES):
        segs = [x[j * TOK:(j + 1) * TOK] for j in range(i)] + [x[j * TOK:(j + 1) * TOK][::-1] for j in range(c.NCORES - 1, i, -1)]
        dirs = [0, 1] + [0] * i + [1] * (c.NCORES - 1 - i)
        m = dict(common)
        m["x_own"] = np.ascontiguousarray(x[i * TOK:(i + 1) * TOK])
        m["x_oth"] = np.ascontiguousarray(np.concatenate(segs, 0)) if segs else np.zeros((TOK, D), np.float32)
        m["w_gs"] = np.ascontiguousarray(np.concatenate([(wgb if d else wgf) for d in dirs], 0))
        m["wgu_s"] = np.ascontiguousarray(np.concatenate([wgu[d] for d in dirs], 0))
        m["bgu_s"] = np.ascontiguousarray(np.stack([bgu[d] for d in dirs], 0))
        m["cm"] = _make_consts(c, i)
        maps.append(m)
    return maps


_CACHE = {}


def kernel(**inputs):
    c = Cfg()
    if "nc" not in _CACHE:
        _CACHE["nc"] = build(c)
    nc = _CACHE["nc"]
    maps = _prep_inputs(c, inputs)
    res = run_bass_kernel_spmd(nc, maps, core_ids=list(range(c.NCORES)))
    outs = [np.asarray(res.results[i]["out"], dtype=np.float32) for i in range(c.NCORES)]
    return np.concatenate(outs, 0).reshape(1, c.SEQ, c.D)
```

```python
import contextlib
import numpy as np
import concourse.bass as bass
import concourse.mybir as mybir
from concourse.bass_utils import run_bass_kernel_spmd

F32 = mybir.dt.float32
BF16 = mybir.dt.bfloat16
I32 = mybir.dt.int32
ALU = mybir.AluOpType
AF = mybir.ActivationFunctionType
AX = mybir.AxisListType

EPS = 1e-6
GATE_TAU = 16.0
CONV_W = 31
TOPK = 8
NGRP = 8
TOPG = 4
RSCALE = 2.5


class _Op:
    __slots__ = ("eng", "fn", "reads", "writes", "dma", "deps", "needed", "cnt", "sem", "target")

    def __init__(self, eng, fn, reads, writes, dma):
        self.eng = eng; self.fn = fn; self.reads = reads; self.writes = writes; self.dma = dma
        self.deps = (); self.needed = False; self.cnt = 0; self.sem = None; self.target = 0


class Sched:
    QUEUES = ("pe", "act", "dve", "pool", "sp")

    def __init__(self, nc, stack, ndma_sems=12):
        self.nc = nc
        self.E = {"pe": nc.tensor, "act": nc.scalar, "dve": nc.vector, "pool": nc.gpsimd, "sp": nc.sync}
        self.ops = []
        self.esem = {q: stack.enter_context(nc.semaphore("e_" + q)) for q in ("pe", "act", "dve", "pool")}
        self.dsem = {q: [stack.enter_context(nc.semaphore("d_%s%d" % (q, i))) for i in range(ndma_sems)]
                     for q in ("sp", "pool")}
        self.cnt = {q: 0 for q in self.esem}
        self.ndma = {q: 0 for q in self.dsem}
        self.hist = {q: [] for q in self.dsem}
        self.stats = dict(ops=0, waits=0)

    def op(self, eng, fn, reads=(), writes=()):
        self.ops.append(_Op(eng, fn, tuple(reads), tuple(writes), False))

    def dma(self, eng, fn, reads=(), writes=()):
        self.ops.append(_Op(eng, fn, tuple(reads), tuple(writes), True))

    def flush(self, barrier=True):
        ops = self.ops
        self.ops = []
        last_w = {}; readers = {}
        for i, o in enumerate(ops):
            d = set()
            for k in o.reads:
                if k in last_w: d.add(last_w[k])
            for k in o.writes:
                if k in last_w: d.add(last_w[k])
                for r in readers.get(k, ()): d.add(r)
            d.discard(i)
            o.deps = d
            for k in o.writes:
                last_w[k] = i; readers[k] = []
            for k in o.reads:
                readers.setdefault(k, []).append(i)
        seen = {q: {} for q in self.QUEUES}
        seen_dma = {q: set() for q in self.QUEUES}
        lastc = {}
        for i, o in enumerate(ops):
            keep = []; byeng = {}
            for j in o.deps:
                p = ops[j]
                if p.dma:
                    if j not in seen_dma[o.eng]: keep.append(j)
                else:
                    if p.eng == "pe" and o.eng == "pe": continue
                    byeng[p.eng] = max(byeng.get(p.eng, -1), j)
            for e, j in byeng.items():
                if seen[o.eng].get(e, -1) >= j: continue
                seen[o.eng][e] = j
                keep.append(j)
            for j in keep:
                ops[j].needed = True
                if ops[j].dma: seen_dma[o.eng].add(j)
            o.deps = sorted(keep)
            if not o.dma: lastc[o.eng] = i
        if barrier:
            for j in lastc.values(): ops[j].needed = True
        for i, o in enumerate(ops):
            eng = self.E[o.eng]
            for j in o.deps:
                p = ops[j]
                if p.dma: eng.wait_ge(p.sem, p.target)
                else: eng.wait_ge(self.esem[p.eng], p.cnt)
                self.stats["waits"] += 1
            if o.dma:
                n = self.ndma[o.eng]; K = len(self.dsem[o.eng])
                o.sem = self.dsem[o.eng][n % K]; o.target = 16 * (n // K + 1)
                if n >= K: eng.wait_ge(o.sem, 16 * (n // K))
                self.ndma[o.eng] = n + 1
                o.fn().then_inc(o.sem, 16)
                self.hist[o.eng].append((o.sem, o.target))
                if len(self.hist[o.eng]) > K: self.hist[o.eng].pop(0)
            else:
                ins = o.fn()
                if o.needed:
                    self.cnt[o.eng] += 1; o.cnt = self.cnt[o.eng]
                    ins.then_inc(self.esem[o.eng], 1)
            self.stats["ops"] += 1
        if barrier:
            for q in self.QUEUES:
                eng = self.E[q]
                for e in self.esem:
                    if e != q and self.cnt[e] > 0: eng.wait_ge(self.esem[e], self.cnt[e])
                for dq in self.dsem:
                    for (sem, tgt) in self.hist[dq]: eng.wait_ge(sem, tgt)


class Cfg:
    def __init__(self, D=4096, SEQ=8192, CTX=256, NCORES=8, E=64, DE=512, DS=512, C=512, GRID_W=64):
        self.D = D; self.SEQ = SEQ; self.CTX = CTX; self.NCORES = NCORES
        self.E = E; self.DE = DE; self.DS = DS; self.C = C; self.GRID_W = GRID_W
        self.TOK = SEQ // NCORES; self.NT = self.TOK // 128; self.KD = D // 128
        self.DG = D // 2; self.NH = self.DG // 128; self.NP = self.NH // 2; self.DK = self.NH * 64
        self.DC = D - self.DG; self.NCC = self.DC // 128
        self.NTB = min(512, self.TOK)
        self.NS = NCORES - 1
        self.CT = CTX // 128
        self.NB = C // 128
        self.NHB = DE // 128; self.NSB = DS // 128
        self.DQ = min(2048, D); self.NQ = D // self.DQ; self.DW = min(512, self.DQ)
        self.EPG = E // NGRP


def _cm_layout(c):
    names = [("ident", 128), ("McF", 128), ("McB", 128), ("M1F", 128), ("M1B", 128), ("maskF2", 256),
             ("maskB2", 256), ("ones", 128), ("ustr", 128), ("iotaC", c.C), ("tokid1", c.NT),
             ("trash", 1), ("flags", 3 * (c.NS + 1)), ("negcol", 1)]
    off = {}; o = 0
    for n, w in names:
        off[n] = (o, w); o += w
    return off, o


def _make_consts(c, core):
    off, tot = _cm_layout(c)
    cm = np.zeros((128, tot), np.float32)
    s = np.arange(128)[:, None]; t = np.arange(128)[None, :]
    g = -1.0 / GATE_TAU
    def put(n, a): cm[:, off[n][0]:off[n][0] + off[n][1]] = a
    put("ident", (s == t))
    put("McF", (s <= t) * g); put("McB", (s >= t) * g)
    put("M1F", (s > t) * g); put("M1B", (s < t) * g)
    mf = (s <= t).astype(np.float32); mb = (s >= t).astype(np.float32)
    put("maskF2", np.concatenate([mf, mf], 1)); put("maskB2", np.concatenate([mb, mb], 1))
    put("ones", np.ones((128, 128))); put("ustr", (s < t))
    put("iotaC", np.broadcast_to(np.arange(c.C)[None, :], (128, c.C)))
    put("tokid1", np.arange(c.NT)[None, :] * 128 + np.arange(128)[:, None] + 1)
    put("trash", c.TOK + np.arange(128)[:, None])
    fl = np.zeros((3 * (c.NS + 1),), np.float32)
    for b in range(c.NS + 1):
        fl[3 * b + 0] = 1.0 if b == core else 0.0
        fl[3 * b + 1] = 0.0 if b == core else 1.0
        fl[3 * b + 2] = 1.0 if b == core else 0.0
    put("flags", np.broadcast_to(fl[None, :], (128, fl.size)))
    put("negcol", np.full((128, 1), g))
    return cm


def build(c, stop=None, dbg=False):
    nc = bass.Bass("TRN2", target_bir_lowering=False)
    off, CMW = _cm_layout(c)
    D, KD, TOK, NT, NTB = c.D, c.KD, c.TOK, c.NT, c.NTB
    DK, DG, DC, NP, NCC, E, C, NB = c.DK, c.DG, c.DC, c.NP, c.NCC, c.E, c.C, c.NB
    TPB = NTB // 128
    KP = min(8, KD)

    def din(name, shape, dt=F32):
        return nc.dram_tensor(name, list(shape), dt, kind="ExternalInput").ap()

    def dscr(name, shape, dt=F32):
        if dbg:
            return nc.dram_tensor(name, list(shape), dt, kind="ExternalOutput").ap()
        return nc.dram_tensor(name, list(shape), dt).ap()

    x_own = din("x_own", [TOK, D]); x_oth = din("x_oth", [max(c.NS, 1) * TOK, D]); ctx2 = din("ctx2", [2 * c.CTX, D])
    cc = din("cc", [2, D]); w_ada = din("w_ada", [D, 6 * D]); b_ada = din("b_ada", [6 * D])
    gvecs = din("gvecs", [3, D])
    w_q = din("w_q", [D, DK]); w_k = din("w_k", [D, DK]); w_v = din("w_v", [D, DG]); w_r = din("w_r", [D, DG])
    w_ab = din("w_ab", [D, 2 * DC]); w_g = din("w_g", [D, 32])
    w_gs = din("w_gs", [(c.NS + 2) * D, 16]); wgu_s = din("wgu_s", [(c.NS + 2) * 16, DK]); bgu_s = din("bgu_s", [c.NS + 2, DK])
    wgu = din("wgu", [32, DK]); bgu = din("bgu", [2, DK])
    g_gla = din("g_gla", [DG]); w_dw = din("w_dw", [CONV_W, DC]); cvec = din("cvec", [3, DC])
    w_out = din("w_out", [D, D]); w_rt = din("w_rt", [D, E]); b_rt = din("b_rt", [E])
    w_eg = din("w_eg", [E * c.NHB * 128, KD * 128]); w_eu = din("w_eu", [E * c.NHB * 128, KD * 128])
    w_ed = din("w_ed", [E * c.DE, D])
    w_sg = din("w_sg", [D, c.DS]); w_su = din("w_su", [D, c.DS]); w_sd = din("w_sd", [c.DS, D])
    cm_in = din("cm", [128, CMW])
    out = nc.dram_tensor("out", [TOK, D], F32, kind="ExternalOutput").ap()

    mod_d = dscr("mod_d", [2, 6 * D])
    qT_d = dscr("qT_d", [NP * 128, TOK], BF16); kT_d = dscr("kT_d", [NP * 128, TOK], BF16)
    k_d = dscr("k_d", [TOK, DK], BF16); v_d = dscr("v_d", [TOK, DG], BF16); sr_d = dscr("sr_d", [TOK, DG], BF16)
    u_d = dscr("u_d", [NCC * 128, TOK], BF16); ob_d = dscr("ob_d", [TOK, DG])
    xlat_d = dscr("xlat_d", [TOK, D]); xs2_d = dscr("xs2_d", [TOK, D], BF16)
    y_q = [dscr("y_q%d" % q, [TOK + 128, c.DQ]) for q in range(c.NQ)]
    NBLKA = (DK + 255) // 256 + (DG + 255) // 256
    wkv_c = nc.dram_tensor("wkv_c", [NBLKA * (KD // KP) * 128, KP * 256], BF16).ap()

    ucnt = [0]

    def uname(n):
        ucnt[0] += 1
        return "s%d_%s" % (ucnt[0], n)

    top = contextlib.ExitStack()
    with top:
        S = Sched(nc, top)
        cm = top.enter_context(nc.sbuf_tensor("s_cm", [128, CMW], F32))
        idb = top.enter_context(nc.sbuf_tensor("s_idb", [128, 128], BF16))
        colA = top.enter_context(nc.sbuf_tensor("s_colA", [128, 4 * KD], F32))
        colB = top.enter_context(nc.sbuf_tensor("s_colB", [128, 4 * KD], F32))
        Gs = top.enter_context(nc.sbuf_tensor("s_Gs", [128, 3, KD], F32))
        gla_scope = contextlib.ExitStack()
        S0 = gla_scope.enter_context(nc.sbuf_tensor("s_S0", [128, 2, NP * 256], F32))
        lowT = gla_scope.enter_context(nc.sbuf_tensor("s_lowT", [16, 2, TOK], BF16))

        def CM(n, lo=0, hi=None):
            o, w = off[n]
            hi = w if hi is None else hi
            return cm[:, o + lo:o + hi]
        ident = CM("ident")
        Gm = Gs[:, 0, :]; cGm = Gs[:, 1, :]; Gf = Gs[:, 2, :]
        shm = colA[:, 2 * KD:3 * KD]; cshm = colB[:, 3 * KD:4 * KD]; shf = colB[:, 2 * KD:3 * KD]

        V = lambda fn, r=(), w=(): S.op("dve", fn, r, w)
        A = lambda fn, r=(), w=(): S.op("act", fn, r, w)
        G = lambda fn, r=(), w=(): S.op("pool", fn, r, w)
        P = lambda fn, r=(), w=(): S.op("pe", fn, r, w)
        DM = lambda fn, r=(), w=(): S.dma("sp", fn, r, w)
        GD = lambda fn, r=(), w=(): S.dma("pool", fn, r, w)
        alt = [0]

        def VA(fv, fa, r=(), w=()):
            alt[0] ^= 1
            if alt[0]: V(fv, r, w)
            else: A(fa, r, w)

        DM(lambda: nc.sync.dma_start(out=cm[:], in_=cm_in), w=["cm"])
        V(lambda: nc.vector.tensor_copy(out=idb[:], in_=ident), r=["cm"], w=["idb"])
        S.flush()

        with contextlib.ExitStack() as ph:
            T = lambda n, s, d=F32: ph.enter_context(nc.sbuf_tensor(uname(n), list(s), d))
            ps = ph.enter_context(nc.psum_tensor("ps0", [128, 8, 512], F32))
            cst = T("cst", [128, 128]); sct = T("sct", [128, 128]); scT2 = T("scT2", [128, KD, 2])
            wst = [T("wst%d" % i, [128, KP, 512]) for i in range(3)]
            brow = [T("brow%d" % i, [2, 512]) for i in range(2)]
            mst = [T("mst%d" % i, [2, 512]) for i in range(2)]
            stk = [T("stk%d" % i, [128, 128]) for i in range(2)]
            V(lambda: nc.vector.memset(cst[:], 0.0), w=["cst"])
            for v in range(2):
                DM(lambda v=v: nc.sync.dma_start(out=cst[v * KD:(v + 1) * KD, :], in_=cc[v].rearrange("(k p) -> k p", p=128)),
                   w=["cst"])
            A(lambda: nc.scalar.activation(out=cst[:], in_=cst[:], func=AF.Silu), r=["cst"], w=["cst"])
            P(lambda: nc.tensor.transpose(out=ps[:, 7, 0:128], in_=cst[:], identity=ident), r=["cst"], w=[("bk", 7)])
            V(lambda: nc.vector.tensor_copy(out=sct[:], in_=ps[:, 7, 0:128]), r=[("bk", 7)], w=["sct"])
            for v in range(2):
                V(lambda v=v: nc.vector.tensor_copy(out=scT2[:, :, v], in_=sct[:, v * KD:(v + 1) * KD]), r=["sct"], w=["scT2"])
            NBLK = 6 * D // 512
            wi = 0
            for j in range(NBLK):
                bank = j % 4
                DM(lambda j=j: nc.sync.dma_start(out=brow[j % 2][:], in_=b_ada[j * 512:(j + 1) * 512].partition_broadcast(2)),
                   w=[("brow", j % 2)])
                for kp in range(KD // KP):
                    r_ = wi % 3; wi += 1
                    DM(lambda j=j, kp=kp, r_=r_: nc.sync.dma_start(
                        out=wst[r_][:], in_=w_ada[kp * KP * 128:(kp + 1) * KP * 128, j * 512:(j + 1) * 512]
                        .rearrange("(k p) n -> p k n", p=128)), w=[("wst", r_)])
                    for kk in range(KP):
                        k = kp * KP + kk
                        P(lambda k=k, kk=kk, r_=r_, bank=bank: nc.tensor.matmul(
                            out=ps[0:2, bank, :], lhsT=scT2[:, k, :], rhs=wst[r_][:, kk, :],
                            start=(k == 0), stop=(k == KD - 1)), r=[("wst", r_), "scT2"], w=[("pb", bank)])
                V(lambda j=j, bank=bank: nc.vector.tensor_tensor(out=mst[j % 2][:], in0=ps[0:2, bank, :], in1=brow[j % 2][:], op=ALU.add),
                  r=[("pb", bank), ("brow", j % 2)], w=[("mst", j % 2)])
                DM(lambda j=j: nc.sync.dma_start(out=mod_d[:, j * 512:(j + 1) * 512], in_=mst[j % 2][:]),
                   r=[("mst", j % 2)], w=["mod_d"])
            rows = lambda ap: ap.rearrange("(k p) -> k p", p=128)
            vecsA = [gvecs[0], mod_d[0, D:2 * D], mod_d[0, 0:D], mod_d[1, D:2 * D]]
            vecsB = [gvecs[1], mod_d[0, 4 * D:5 * D], mod_d[0, 3 * D:4 * D], mod_d[1, 0:D]]
            for si, (vecs, col) in enumerate(((vecsA, colA), (vecsB, colB))):
                V(lambda si=si: nc.vector.memset(stk[si][:], 0.0), w=[("stk", si)])
                for vi, vap in enumerate(vecs):
                    DM(lambda si=si, vi=vi, vap=vap: nc.sync.dma_start(out=stk[si][vi * KD:(vi + 1) * KD, :], in_=rows(vap)),
                       r=["mod_d"], w=[("stk", si)])
                P(lambda si=si: nc.tensor.transpose(out=ps[:, 6, si * 128:(si + 1) * 128], in_=stk[si][:], identity=ident),
                  r=[("stk", si)], w=[("bk", 6)])
                V(lambda si=si, col=col: nc.vector.tensor_copy(out=col[:], in_=ps[:, 6, si * 128:si * 128 + 4 * KD]),
                  r=[("bk", 6)], w=[("col", si)])
            V(lambda: nc.vector.scalar_tensor_tensor(out=Gm, in0=colA[:, KD:2 * KD], scalar=1.0, in1=colA[:, 0:KD], op0=ALU.add, op1=ALU.mult),
              r=[("col", 0)], w=["Gs0"])
            V(lambda: nc.vector.scalar_tensor_tensor(out=cGm, in0=colA[:, 3 * KD:4 * KD], scalar=1.0, in1=colA[:, 0:KD], op0=ALU.add, op1=ALU.mult),
              r=[("col", 0)], w=["Gs1"])
            V(lambda: nc.vector.scalar_tensor_tensor(out=Gf, in0=colB[:, KD:2 * KD], scalar=1.0, in1=colB[:, 0:KD], op0=ALU.add, op1=ALU.mult),
              r=[("col", 1)], w=["Gs2"])
            S.flush()
            if stop == "P0":
                return nc

        def norm_tile(Tn, ps, src_ap, Gc, shc, dst, idx, xs2_dst=None, h2f=None, tpbanks=(4,)):
            r_ = idx % 2
            xt = Tn["xt"][r_]; ss = Tn["ss"]; rs = Tn["rs"]
            DM(lambda: nc.sync.dma_start(out=xt[:], in_=src_ap), w=[("xt", r_)])
            G(lambda: nc.gpsimd.memset(ss[:, r_:r_ + 1], 0.0), w=[("ss", r_)])
            A(lambda: nc.scalar.activation(out=Tn["junk"][:], in_=xt[:], func=AF.Square, scale=float(D) ** -0.5,
                                           accum_out=ss[:, r_:r_ + 1]), r=[("xt", r_), ("ss", r_)], w=["junk", ("ss", r_)])
            A(lambda: nc.scalar.activation(out=rs[:, r_:r_ + 1], in_=ss[:, r_:r_ + 1], func=AF.Sqrt, bias=EPS, scale=1.0),
              r=[("ss", r_)], w=[("rs", r_)])
            V(lambda: nc.vector.reciprocal(out=rs[:, r_:r_ + 1], in_=rs[:, r_:r_ + 1]), r=[("rs", r_)], w=[("rs", r_)])
            V(lambda: nc.vector.tensor_scalar(out=xt[:], in0=xt[:], scalar1=rs[:, r_:r_ + 1], scalar2=None, op0=ALU.mult),
              r=[("xt", r_), ("rs", r_)], w=[("xt", r_)])
            if xs2_dst is not None:
                xb = Tn["xb"]
                G(lambda: nc.gpsimd.tensor_copy(out=xb[:], in_=xt[:]), r=[("xt", r_)], w=["xb"])
                DM(lambda: nc.sync.dma_start(out=xs2_dst, in_=xb[:]), r=["xb"], w=["xs2_d"])
            for k in range(KD):
                if k % 4 == 0:
                    Tn["tq"][0] += 1
                tpbank = tpbanks[Tn["tq"][0] % len(tpbanks)]
                q = k % 4
                pa = ps[:, tpbank, q * 128:(q + 1) * 128]
                P(lambda k=k, pa=pa: nc.tensor.transpose(out=pa, in_=xt[:, k * 128:(k + 1) * 128], identity=ident),
                  r=[("xt", r_)], w=[("bk", tpbank)])
                d_ = dst(k)
                if h2f is None:
                    VA(lambda k=k, pa=pa, d_=d_: nc.vector.tensor_scalar(out=d_[0], in0=pa, scalar1=Gc[:, k:k + 1], scalar2=shc[:, k:k + 1],
                                                                         op0=ALU.mult, op1=ALU.add),
                       lambda k=k, pa=pa, d_=d_: nc.scalar.activation(out=d_[0], in_=pa, func=AF.Identity, scale=Gc[:, k:k + 1],
                                                                      bias=shc[:, k:k + 1]),
                       r=[("bk", tpbank)], w=[d_[1]])
                else:
                    h_ = h2f(k)
                    VA(lambda k=k, pa=pa, h_=h_: nc.vector.tensor_scalar(out=h_[0], in0=pa, scalar1=Gc[:, k:k + 1], scalar2=shc[:, k:k + 1],
                                                                         op0=ALU.mult, op1=ALU.add),
                       lambda k=k, pa=pa, h_=h_: nc.scalar.activation(out=h_[0], in_=pa, func=AF.Identity, scale=Gc[:, k:k + 1],
                                                                      bias=shc[:, k:k + 1]),
                       r=[("bk", tpbank)], w=[h_[1]])
                    G(lambda d_=d_, h_=h_: nc.gpsimd.tensor_copy(out=d_[0], in_=h_[0]), r=[h_[1]], w=[d_[1]])

        def norm_bufs(T, with_xb=False):
            Tn = {"xt": [T("xt0", [128, D]), T("xt1", [128, D])], "junk": T("junk", [128, D], BF16),
                  "ss": T("ss", [128, 2]), "rs": T("rs", [128, 2]), "tq": [0]}
            if with_xb: Tn["xb"] = T("xb", [128, D], BF16)
            return Tn

        def gates(Tg, ps, lo_ap, wg_b, bg_b, M1, key, zbanks=(6, 7)):
            sp = Tg["sp"]; Ek = Tg["Ek"]; zb = Tg["zb"]
            for hh in range(0, DK, 512):
                w_ = min(512, DK - hh); bank = zbanks[(hh // 512) % 2]
                P(lambda hh=hh, w_=w_, bank=bank: nc.tensor.matmul(out=ps[:, bank, 0:w_], lhsT=lo_ap, rhs=wg_b[:, hh:hh + w_], start=True, stop=True),
                  r=[key, "wg_b"], w=[("zb", bank)])
                V(lambda hh=hh, w_=w_, bank=bank: nc.vector.tensor_tensor(out=zb[:, hh:hh + w_], in0=ps[:, bank, 0:w_], in1=bg_b[:, hh:hh + w_], op=ALU.add),
                  r=[("zb", bank), "bg_b"], w=["zbuf"])
            A(lambda: nc.scalar.activation(out=zb[:], in_=zb[:], func=AF.Exp, scale=-1.0), r=["zbuf"], w=["zbuf"])
            A(lambda: nc.scalar.activation(out=sp[:], in_=zb[:], func=AF.Ln, bias=1.0, scale=1.0), r=["zbuf"], w=["sp"])
            for hh in range(0, DK, 512):
                w_ = min(512, DK - hh); bank = zbanks[(hh // 512) % 2]
                P(lambda hh=hh, w_=w_, bank=bank: nc.tensor.matmul(out=ps[:, bank, 0:w_], lhsT=M1, rhs=sp[:, hh:hh + w_], start=True, stop=True),
                  r=["sp"], w=[("zb", bank)])
                A(lambda hh=hh, w_=w_, bank=bank: nc.scalar.activation(out=Ek[:, hh:hh + w_], in_=ps[:, bank, 0:w_], func=AF.Exp),
                  r=[("zb", bank)], w=["Ek"])

        def state_update(Tg, ps, Sblk, khat, v_ap, ebL, skey, region, bkey):
            for p in range(NP):
                P(lambda p=p: nc.tensor.matmul(out=region, lhsT=khat[:, p * 128:(p + 1) * 128], rhs=v_ap(p), start=True, stop=True),
                  r=["khat", "v_tm"], w=[bkey])
                for hb in range(2):
                    sl = Sblk[hb * 64:(hb + 1) * 64, p * 256 + hb * 128:p * 256 + (hb + 1) * 128]
                    V(lambda p=p, hb=hb, sl=sl: nc.vector.scalar_tensor_tensor(
                        out=sl, in0=sl, scalar=ebL[hb * 64:(hb + 1) * 64, p:p + 1],
                        in1=region[hb * 64:(hb + 1) * 64, hb * 128:(hb + 1) * 128], op0=ALU.mult, op1=ALU.add),
                      r=[bkey, "ebL", skey], w=[skey])

        def load_cast_w(Tw, src_fn, nk, ncols, key):
            r_ = Tw["wi"][0] % len(Tw["wb"]); Tw["wi"][0] += 1
            wb = Tw["wb"][r_]
            for k0 in range(0, nk, KP):
                k1 = min(nk, k0 + KP)
                s_ = Tw["si"][0] % len(Tw["st"]); Tw["si"][0] += 1
                stg = Tw["st"][s_]
                DM(lambda k0=k0, k1=k1, stg=stg: nc.sync.dma_start(out=stg[:, 0:k1 - k0, 0:ncols],
                                                                   in_=src_fn(k0, k1).rearrange("(k p) n -> p k n", p=128)),
                   w=[("wstg", key, s_)])
                if Tw["si"][0] % 2:
                    V(lambda k0=k0, k1=k1, stg=stg, wb=wb: nc.vector.tensor_copy(out=wb[:, k0:k1, 0:ncols], in_=stg[:, 0:k1 - k0, 0:ncols]),
                      r=[("wstg", key, s_)], w=[("wb", key, r_)])
                else:
                    A(lambda k0=k0, k1=k1, stg=stg, wb=wb: nc.scalar.copy(out=wb[:, k0:k1, 0:ncols], in_=stg[:, 0:k1 - k0, 0:ncols]),
                      r=[("wstg", key, s_)], w=[("wb", key, r_)])
            return wb, ("wb", key, r_)

        with contextlib.ExitStack() as ph:
            T = lambda n, s, d=F32: ph.enter_context(nc.sbuf_tensor(uname(n), list(s), d))
            ps = ph.enter_context(nc.psum_tensor("psA", [128, 8, 512], F32))
            Tn = norm_bufs(T)
            hT = T("hT", [128, KD, NTB], BF16)
            k_tm = T("k_tm", [128, TPB, DK], BF16); v_tm = T("v_tm", [128, TPB, DG], BF16)
            stA = [T("stA%d" % i, [128, KP, 256]) for i in range(2)]
            wbA = [T("wbA%d" % i, [128, KP, 256], BF16) for i in range(4)]
            first_pass = [True]
            wgsf = T("wgsf", [128, KD, 16]); wgsb = T("wgsb", [128, KD, 16], BF16)
            wguf = T("wguf", [16, DK]); wgub = T("wgub", [16, DK], BF16); bgb = T("bgb", [128, DK])
            loA = T("loA", [16, NTB], BF16)
            Tg = {"sp": T("sp", [128, DK]), "Ek": T("Ek", [128, DK]), "zb": T("zb", [128, DK])}
            khat = T("khat", [128, DK], BF16); ebL = T("ebL", [128, NP])
            Sb = T("Sb", [128, NP * 256]); Sctxb = T("Sctxb", [128, NP * 256])
            si_ = [0]; stc = [0]

            def slot(src_rows, ntiles, Gc, shc, sidx):
                DM(lambda: nc.sync.dma_start(out=wgsf[:], in_=w_gs[sidx * D:(sidx + 1) * D, :].rearrange("(k p) n -> p k n", p=128)), w=["wgsf"])
                V(lambda: nc.vector.tensor_copy(out=wgsb[:], in_=wgsf[:]), r=["wgsf"], w=["wgsb"])
                DM(lambda: nc.sync.dma_start(out=wguf[:], in_=wgu_s[sidx * 16:(sidx + 1) * 16, :]), w=["wguf"])
                V(lambda: nc.vector.tensor_copy(out=wgub[:], in_=wguf[:]), r=["wguf"], w=["wg_b"])
                DM(lambda: nc.sync.dma_start(out=bgb[:], in_=bgu_s[sidx].partition_broadcast(128)), w=["bg_b"])
                for h0 in range(0, ntiles, TPB):
                    nth = min(TPB, ntiles - h0)
                    for j in range(nth):
                        norm_tile(Tn, ps, src_rows(h0 + j), Gc, shc,
                                  lambda k, j=j: (hT[:, k, j * 128:(j + 1) * 128], ("hT", k, j)), si_[0], tpbanks=(4,))
                        si_[0] += 1
                    hkeys = lambda k: [("hT", k, j) for j in range(nth)]
                    for k in range(KD):
                        P(lambda k=k, nth=nth: nc.tensor.matmul(out=ps[0:16, 5, 0:nth * 128], lhsT=wgsb[:, k, :], rhs=hT[:, k, 0:nth * 128],
                                                       start=(k == 0), stop=(k == KD - 1)), r=["wgsb"] + hkeys(k), w=[("bk", 5)])
                    V(lambda nth=nth: nc.vector.tensor_copy(out=loA[:, 0:nth * 128], in_=ps[0:16, 5, 0:nth * 128]), r=[("bk", 5)], w=["loA"])
                    blocks = [(w_k, c0, min(256, DK - c0), k_tm) for c0 in range(0, DK, 256)] + \
                             [(w_v, c0, min(256, DG - c0), v_tm) for c0 in range(0, DG, 256)]
                    for bi, (wsrc, c0, bw, dst) in enumerate(blocks):
                        half = (bi % 2) * 256
                        for k0 in range(0, KD, KP):
                            s_ = stc[0] % 4; stc[0] += 1
                            crow = (bi * (KD // KP) + k0 // KP) * 128
                            if first_pass[0]:
                                g_ = s_ % 2
                                DM(lambda wsrc=wsrc, c0=c0, bw=bw, k0=k0, g_=g_: nc.sync.dma_start(
                                    out=stA[g_][:, :, 0:bw], in_=wsrc[k0 * 128:(k0 + KP) * 128, c0:c0 + bw].rearrange("(k p) n -> p k n", p=128)),
                                   w=[("stA", g_)])
                                VA(lambda s_=s_, g_=g_, bw=bw: nc.vector.tensor_copy(out=wbA[s_][:, :, 0:bw], in_=stA[g_][:, :, 0:bw]),
                                   lambda s_=s_, g_=g_, bw=bw: nc.scalar.copy(out=wbA[s_][:, :, 0:bw], in_=stA[g_][:, :, 0:bw]),
                                   r=[("stA", g_)], w=[("wbA", s_)])
                                DM(lambda s_=s_, crow=crow: nc.sync.dma_start(out=wkv_c[crow:crow + 128, :], in_=wbA[s_][:].rearrange("p k n -> p (k n)")),
                                   r=[("wbA", s_)], w=[("wkvc", crow)])
                            else:
                                DM(lambda s_=s_, crow=crow: nc.sync.dma_start(out=wbA[s_][:].rearrange("p k n -> p (k n)"), in_=wkv_c[crow:crow + 128, :]),
                                   r=[("wkvc", crow)], w=[("wbA", s_)])
                            for j in range(nth):
                                for kk in range(KP):
                                    k = k0 + kk
                                    P(lambda j=j, k=k, kk=kk, s_=s_, half=half, bw=bw: nc.tensor.matmul(
                                        out=ps[:, j, half:half + bw], lhsT=hT[:, k, j * 128:(j + 1) * 128], rhs=wbA[s_][:, kk, 0:bw],
                                        start=(k == 0), stop=(k == KD - 1)), r=[("wbA", s_), ("hT", k, j)], w=[("bk", j)])
                        for j in range(nth):
                            VA(lambda j=j, dst=dst, c0=c0, bw=bw, half=half: nc.vector.tensor_copy(out=dst[:, j, c0:c0 + bw], in_=ps[:, j, half:half + bw]),
                               lambda j=j, dst=dst, c0=c0, bw=bw, half=half: nc.scalar.copy(out=dst[:, j, c0:c0 + bw], in_=ps[:, j, half:half + bw]),
                               r=[("bk", j)], w=["v_tm" if dst is v_tm else "k_tm"])
                    for j in range(nth):
                        gates(Tg, ps, loA[:, j * 128:(j + 1) * 128], wgub, bgb, CM("M1F"), "loA")
                        V(lambda j=j: nc.vector.tensor_tensor(out=khat[:], in0=k_tm[:, j, :], in1=Tg["Ek"][:], op=ALU.mult),
                          r=["k_tm", "Ek"], w=["khat"])
                        for p in range(NP):
                            P(lambda p=p: nc.tensor.matmul(out=ps[:, 5, 480 + p:481 + p], lhsT=Tg["sp"][:, p * 128:(p + 1) * 128],
                                                           rhs=CM("negcol"), start=True, stop=True), r=["sp"], w=[("bk", 5)])
                        A(lambda: nc.scalar.activation(out=ebL[:], in_=ps[:, 5, 480:480 + NP], func=AF.Exp), r=[("bk", 5)], w=["ebL"])
                        state_update(Tg, ps, Sb, khat, lambda p, j=j: v_tm[:, j, p * 256:(p + 1) * 256], ebL, "Sb", ps[:, 4, 256:512], ("bk", 4))
                    first_pass[0] = False

            V(lambda: nc.vector.memset(Sb[:], 0.0), w=["Sb"])
            slot(lambda t: ctx2[c.CTX + t * 128:c.CTX + (t + 1) * 128, :], c.CT, cGm, cshm, 1)
            V(lambda: nc.vector.tensor_copy(out=Sctxb[:], in_=Sb[:]), r=["Sb"], w=["Sctxb"])
            V(lambda: nc.vector.memset(Sb[:], 0.0), r=["Sctxb"], w=["Sb"])
            V(lambda: nc.vector.memset(S0[:, 0, :], 0.0), w=["S0f"])
            slot(lambda t: ctx2[t * 128:(t + 1) * 128, :], c.CT, cGm, cshm, 0)
            for b in range(c.NS + 1):
                fo = off["flags"][0] + 3 * b
                V(lambda fo=fo: nc.vector.scalar_tensor_tensor(out=S0[:, 0, :], in0=Sb[:], scalar=cm[:, fo:fo + 1], in1=S0[:, 0, :],
                                                               op0=ALU.mult, op1=ALU.add), r=["Sb", "S0f"], w=["S0f"])
                for hf in range(2):
                    cs_ = slice(hf * DK, (hf + 1) * DK)
                    V(lambda fo=fo, cs_=cs_: nc.vector.tensor_scalar(out=Tg["zb"][:], in0=Sctxb[:, cs_], scalar1=cm[:, fo + 2:fo + 3], scalar2=None, op0=ALU.mult),
                      r=["Sctxb", "zbuf"], w=["zbuf"])
                    V(lambda fo=fo, cs_=cs_: nc.vector.scalar_tensor_tensor(out=Sb[:, cs_], in0=Sb[:, cs_], scalar=cm[:, fo + 1:fo + 2], in1=Tg["zb"][:],
                                                                           op0=ALU.mult, op1=ALU.add), r=["Sb", "zbuf", "S0f"], w=["Sb"])
                if b < c.NS:
                    slot(lambda t, b=b: x_oth[b * TOK + t * 128:b * TOK + (t + 1) * 128, :], NT, Gm, shm, 2 + b)
            V(lambda: nc.vector.tensor_copy(out=S0[:, 1, :], in_=Sb[:]), r=["Sb"], w=["S0b"])
            if dbg:
                dS = nc.dram_tensor("dbg_S0", [128, 2 * NP * 256], F32, kind="ExternalOutput").ap()
                DM(lambda: nc.sync.dma_start(out=dS, in_=S0[:].rearrange("p a n -> p (a n)")), r=["S0f", "S0b"])
            S.flush()
            if stop == "A":
                return nc

        with contextlib.ExitStack() as ph:
            T = lambda n, s, d=F32: ph.enter_context(nc.sbuf_tensor(uname(n), list(s), d))
            ps = ph.enter_context(nc.psum_tensor("psB", [128, 8, 512], F32))
            Tn = norm_bufs(T)
            hT = T("hTo", [128, KD, TOK], BF16)
            Tw = {"st": [T("stB%d" % i, [128, KP, 256]) for i in range(3)], "wb": [T("wbB%d" % i, [128, KD, 256], BF16) for i in range(2)],
                  "wi": [0], "si": [0]}
            evb = [T("evb%d" % i, [128, 512], BF16) for i in range(4)]
            evf = [T("evf%d" % i, [128, 512]) for i in range(2)]
            ec = [0]; bk = [0]
            import os
            lvl = int(os.environ.get('KDBG_B2', '9'))
            for t in range(NT if lvl >= 0 else 0):
                norm_tile(Tn, ps, x_own[t * 128:(t + 1) * 128, :], Gm, shm,
                          lambda k, t=t: (hT[:, k, t * 128:(t + 1) * 128], ("hT", k, t)), t, tpbanks=(6, 7))
            hk_all = lambda k: [("hT", k, t) for t in range(NT)]

            def nbank():
                b = bk[0] % 6; bk[0] += 1
                return b

            def fm_group(wb, wkey, c_lo, c_hi, tb, bank, rows=128):
                for k in range(KD):
                    P(lambda k=k: nc.tensor.matmul(out=ps[0:rows, bank, 0:NTB], lhsT=wb[:, k, c_lo:c_hi], rhs=hT[:, k, tb * NTB:(tb + 1) * NTB],
                                                   start=(k == 0), stop=(k == KD - 1)), r=[wkey] + hk_all(k), w=[("pb", bank)])

            def tm_group(wb, wkey, bw, t, bank):
                for k in range(KD):
                    P(lambda k=k: nc.tensor.matmul(out=ps[:, bank, 0:bw], lhsT=hT[:, k, t * 128:(t + 1) * 128], rhs=wb[:, k, 0:bw],
                                                   start=(k == 0), stop=(k == KD - 1)), r=[wkey, ("hT", k, t)], w=[("pb", bank)])

            def evac_bf(bank, n, dram_ap, scale=None, func=None):
                e_ = ec[0] % 4; ec[0] += 1
                if func is not None:
                    A(lambda: nc.scalar.activation(out=evb[e_][:, 0:n], in_=ps[:, bank, 0:n], func=func), r=[("pb", bank)], w=[("evb", e_)])
                elif scale is not None:
                    A(lambda: nc.scalar.mul(out=evb[e_][:, 0:n], in_=ps[:, bank, 0:n], mul=scale), r=[("pb", bank)], w=[("evb", e_)])
                else:
                    VA(lambda: nc.vector.tensor_copy(out=evb[e_][:, 0:n], in_=ps[:, bank, 0:n]),
                       lambda: nc.scalar.copy(out=evb[e_][:, 0:n], in_=ps[:, bank, 0:n]), r=[("pb", bank)], w=[("evb", e_)])
                DM(lambda: nc.sync.dma_start(out=dram_ap, in_=evb[e_][:, 0:n]), r=[("evb", e_)], w=["scr"])

            for (wsrc, dstT, scl) in ((w_q, qT_d, 0.125), (w_k, kT_d, None))[:max(0, lvl)]:
                for c0 in range(0, DK, 256):
                    bw = min(256, DK - c0)
                    wb, wkey = load_cast_w(Tw, lambda k0, k1, wsrc=wsrc, c0=c0, bw=bw: wsrc[k0 * 128:k1 * 128, c0:c0 + bw], KD, bw, "B")
                    for sub in range(bw // 128):
                        for tb in range(TOK // NTB):
                            bank = nbank()
                            fm_group(wb, wkey, sub * 128, (sub + 1) * 128, tb, bank)
                            evac_bf(bank, NTB, dstT[c0 + sub * 128:c0 + (sub + 1) * 128, tb * NTB:(tb + 1) * NTB], scale=scl)
                    if wsrc is w_k:
                        for t in range(NT):
                            bank = nbank()
                            tm_group(wb, wkey, bw, t, bank)
                            evac_bf(bank, bw, k_d[t * 128:(t + 1) * 128, c0:c0 + bw])
            for (wsrc, dst, fn) in ((w_v, v_d, None), (w_r, sr_d, AF.Silu))[:max(0, lvl - 2)]:
                for c0 in range(0, DG, 256):
                    bw = min(256, DG - c0)
                    wb, wkey = load_cast_w(Tw, lambda k0, k1, wsrc=wsrc, c0=c0, bw=bw: wsrc[k0 * 128:k1 * 128, c0:c0 + bw], KD, bw, "B")
                    for t in range(NT):
                        bank = nbank()
                        tm_group(wb, wkey, bw, t, bank)
                        evac_bf(bank, bw, dst[t * 128:(t + 1) * 128, c0:c0 + bw], func=fn)
            for cch in range(NCC if lvl >= 5 else 0):
                wb, wkey = load_cast_w(Tw, lambda k0, k1, cch=cch: w_ab[k0 * 128:k1 * 128, cch * 256:(cch + 1) * 256], KD, 256, "B")
                for tb in range(TOK // NTB):
                    ba = nbank(); fm_group(wb, wkey, 0, 128, tb, ba)
                    bb = nbank(); fm_group(wb, wkey, 128, 256, tb, bb)
                    f_ = ec[0] % 2; e_ = ec[0] % 4; ec[0] += 1
                    A(lambda bb=bb, f_=f_: nc.scalar.activation(out=evf[f_][:, 0:NTB], in_=ps[:, bb, 0:NTB], func=AF.Sigmoid),
                      r=[("pb", bb)], w=[("evf", f_)])
                    V(lambda ba=ba, f_=f_, e_=e_: nc.vector.tensor_tensor(out=evb[e_][:, 0:NTB], in0=ps[:, ba, 0:NTB], in1=evf[f_][:, 0:NTB], op=ALU.mult),
                      r=[("pb", ba), ("evf", f_)], w=[("evb", e_)])
                    DM(lambda cch=cch, tb=tb, e_=e_: nc.sync.dma_start(out=u_d[cch * 128:(cch + 1) * 128, tb * NTB:(tb + 1) * NTB], in_=evb[e_][:, 0:NTB]),
                       r=[("evb", e_)], w=["scr"])
            if lvl >= 6:
                wb, wkey = load_cast_w(Tw, lambda k0, k1: w_g[k0 * 128:k1 * 128, 0:32], KD, 32, "B")
            for d_ in range(2 if lvl >= 6 else 0):
                for tb in range(TOK // NTB):
                    bank = nbank()
                    fm_group(wb, wkey, d_ * 16, (d_ + 1) * 16, tb, bank, rows=16)
                    V(lambda d_=d_, tb=tb, bank=bank: nc.vector.tensor_copy(out=lowT[:, d_, tb * NTB:(tb + 1) * NTB], in_=ps[0:16, bank, 0:NTB]),
                      r=[("pb", bank)], w=["lowT"])
            S.flush()
            if stop == "B2":
                return nc

        own = contextlib.ExitStack()
        with own:
            actT = own.enter_context(nc.sbuf_tensor("s_actT", [128, KD, TOK], BF16))
            with contextlib.ExitStack() as ph:
                T = lambda n, s, d=F32: ph.enter_context(nc.sbuf_tensor(uname(n), list(s), d))
                ps = ph.enter_context(nc.psum_tensor("psG", [128, 8, 512], F32))
                kt = [T("kt%d" % i, [128, DK], BF16) for i in range(2)]
                vt = [T("vt%d" % i, [128, DG], BF16) for i in range(2)]
                srt = [T("srt%d" % i, [128, DG], BF16) for i in range(2)]
                obt = [T("obt%d" % i, [128, DG]) for i in range(2)]
                qTt = [T("qTt%d" % i, [128, NP, 128], BF16) for i in range(2)]
                kTt = [T("kTt%d" % i, [128, NP, 128], BF16) for i in range(2)]
                wgub = T("wgub2", [16, 2, DK], BF16); bgb = T("bgb2", [128, 2, DK])
                Tg = {"sp": T("sp2", [128, DK]), "Ek": T("Ek2", [128, DK]), "zb": T("zb2", [128, DK])}
                khat = T("khat2", [128, DK], BF16)
                Eb = [T("Eb%d" % i, [128, 128]) for i in range(2)]; Enb = [T("Enb%d" % i, [128, 128]) for i in range(2)]
                qd = [T("qd%d" % i, [128, 128], BF16) for i in range(2)]; kd = [T("kd%d" % i, [128, 128], BF16) for i in range(2)]
                Qblk = [T("Qblk%d" % i, [128, 256], BF16) for i in range(2)]
                att = [T("att%d" % i, [128, 256], BF16) for i in range(2)]
                Sw = T("Sw", [128, NP * 256]); Sbf = T("Sbf", [128, NP * 256], BF16)
                obuf = obt[0]; osum = [T("osum%d" % i, [128, 256]) for i in range(2)]
                gsb = [T("gsb%d" % i, [128, 256]) for i in range(2)]; onb = [T("onb%d" % i, [128, 256]) for i in range(2)]
                ggb = T("ggb", [128, DG]); ssh = T("ssh", [128, 4]); rsh = T("rsh", [128, 4]); junk2 = T("junk2", [128, 128])
                for d_ in range(2):
                    DM(lambda d_=d_: nc.sync.dma_start(out=Tg["zb"][0:16, :], in_=wgu[d_ * 16:(d_ + 1) * 16, :]), w=["zbuf"])
                    V(lambda d_=d_: nc.vector.tensor_copy(out=wgub[:, d_, :], in_=Tg["zb"][0:16, :]), r=["zbuf"], w=["wg_b"])
                for d_ in range(2):
                    DM(lambda d_=d_: nc.sync.dma_start(out=bgb[:, d_, :], in_=bgu[d_].partition_broadcast(128)), w=["bg_b"])
                DM(lambda: nc.sync.dma_start(out=ggb[:], in_=g_gla.partition_broadcast(128)), w=["ggb"])
                for i in range(2):
                    V(lambda i=i: nc.vector.memset(Qblk[i][:], 0.0), w=[("Qblk", i)])
                uc = [0]
                for dirn in (1, 0):
                    Mc = CM("McB") if dirn else CM("McF"); M1 = CM("M1B") if dirn else CM("M1F")
                    mask2 = CM("maskB2") if dirn else CM("maskF2")
                    V(lambda dirn=dirn: nc.vector.tensor_copy(out=Sw[:], in_=S0[:, dirn, :]), r=["Sw", "Sbf"], w=["Sw"])
                    G(lambda: nc.gpsimd.tensor_copy(out=Sbf[:], in_=Sw[:]), r=["Sw"], w=["Sbf"])
                    order = range(NT - 1, -1, -1) if dirn else range(NT)
                    for ti, t in enumerate(order):
                        r_ = ti % 2
                        rows = slice(t * 128, (t + 1) * 128)
                        DM(lambda r_=r_, rows=rows: nc.sync.dma_start(out=kt[r_][:], in_=k_d[rows, :]), w=[("kt", r_)])
                        DM(lambda r_=r_, rows=rows: nc.sync.dma_start(out=vt[r_][:], in_=v_d[rows, :]), w=[("vt", r_)])
                        DM(lambda r_=r_, rows=rows: nc.sync.dma_start(out=qTt[r_][:], in_=qT_d.rearrange("(p r) t -> r p t", r=128)[:, :, rows]),
                           w=[("qTt", r_)])
                        DM(lambda r_=r_, rows=rows: nc.sync.dma_start(out=kTt[r_][:], in_=kT_d.rearrange("(p r) t -> r p t", r=128)[:, :, rows]),
                           w=[("kTt", r_)])
                        if not dirn:
                            DM(lambda r_=r_, rows=rows: nc.sync.dma_start(out=srt[r_][:], in_=sr_d[rows, :]), w=[("srt", r_)])
                            DM(lambda r_=r_, rows=rows: nc.sync.dma_start(out=obt[r_][:], in_=ob_d[rows, :]), r=[("ob_d", t), "obuf"], w=[("obt", r_), "obuf"])
                        gates(Tg, ps, lowT[:, dirn, rows], wgub[:, dirn, :], bgb[:, dirn, :], M1, "lowT")
                        V(lambda r_=r_: nc.vector.tensor_tensor(out=khat[:], in0=kt[r_][:], in1=Tg["Ek"][:], op=ALU.mult),
                          r=[("kt", r_), "Ek"], w=["khat"])
                        for p in range(NP):
                            u_ = uc[0] % 2; uc[0] += 1
                            q4 = 0
                            bTp = ps[:, 0, 0:128]
                            P(lambda p=p, bTp=bTp, Mc=Mc: nc.tensor.matmul(out=bTp, lhsT=Tg["sp"][:, p * 128:(p + 1) * 128], rhs=Mc, start=True, stop=True),
                              r=["sp"], w=[("bk", 0)])
                            A(lambda u_=u_, bTp=bTp: nc.scalar.activation(out=Eb[u_][:], in_=bTp, func=AF.Exp), r=[("bk", 0)], w=[("Eb", u_)])
                            A(lambda u_=u_, bTp=bTp: nc.scalar.activation(out=Enb[u_][:], in_=bTp, func=AF.Exp, scale=-1.0), r=[("bk", 0)], w=[("Enb", u_)])
                            V(lambda p=p, u_=u_, r_=r_: nc.vector.tensor_tensor(out=qd[u_][:], in0=qTt[r_][:, p, :], in1=Eb[u_][:], op=ALU.mult),
                              r=[("qTt", r_), ("Eb", u_)], w=[("qd", u_)])
                            G(lambda p=p, u_=u_, r_=r_: nc.gpsimd.tensor_tensor(out=kd[u_][:], in0=kTt[r_][:, p, :], in1=Enb[u_][:], op=ALU.mult),
                              r=[("kTt", r_), ("Enb", u_)], w=[("kd", u_)])
                            for hb in range(2):
                                G(lambda u_=u_, hb=hb: nc.gpsimd.tensor_copy(out=Qblk[u_][hb * 64:(hb + 1) * 64, hb * 128:(hb + 1) * 128],
                                                                             in_=qd[u_][hb * 64:(hb + 1) * 64, :]), r=[("qd", u_)], w=[("Qblk", u_)])
                            ap_ = ps[:, 1 + u_, 0:256]
                            P(lambda u_=u_, ap_=ap_: nc.tensor.matmul(out=ap_, lhsT=kd[u_][:], rhs=Qblk[u_][:], start=True, stop=True),
                              r=[("kd", u_), ("Qblk", u_)], w=[("bk", 1 + u_)])
                            V(lambda u_=u_, ap_=ap_, mask2=mask2: nc.vector.tensor_tensor(out=att[u_][:], in0=ap_, in1=mask2, op=ALU.mult),
                              r=[("bk", 1 + u_)], w=[("att", u_)])
                            op_ = ps[:, 3 + u_, 0:256]
                            P(lambda p=p, u_=u_, op_=op_: nc.tensor.matmul(out=op_, lhsT=qd[u_][:], rhs=Sbf[:, p * 256:(p + 1) * 256], start=True, stop=False),
                              r=[("qd", u_), "Sbf"], w=[("bk", 3 + u_)])
                            for hb in range(2):
                                P(lambda p=p, u_=u_, hb=hb, r_=r_: nc.tensor.matmul(
                                    out=ps[:, 3 + u_, hb * 128:(hb + 1) * 128], lhsT=att[u_][:, hb * 128:(hb + 1) * 128],
                                    rhs=vt[r_][:, p * 256 + hb * 128:p * 256 + (hb + 1) * 128], start=False, stop=(hb == 1)),
                                  r=[("att", u_), ("vt", r_)], w=[("bk", 3 + u_)])
                            ecol = 0 if dirn else 127
                            P(lambda p=p, r_=r_: nc.tensor.matmul(out=ps[:, 5, 0:256], lhsT=khat[:, p * 128:(p + 1) * 128],
                                                                   rhs=vt[r_][:, p * 256:(p + 1) * 256], start=True, stop=True),
                              r=["khat", ("vt", r_)], w=[("bk", 5)])
                            for hb in range(2):
                                sl = Sw[hb * 64:(hb + 1) * 64, p * 256 + hb * 128:p * 256 + (hb + 1) * 128]
                                V(lambda hb=hb, sl=sl, u_=u_, ecol=ecol: nc.vector.scalar_tensor_tensor(
                                    out=sl, in0=sl, scalar=Eb[u_][hb * 64:(hb + 1) * 64, ecol:ecol + 1],
                                    in1=ps[hb * 64:(hb + 1) * 64, 5, hb * 128:(hb + 1) * 128], op0=ALU.mult, op1=ALU.add),
                                  r=[("bk", 5), ("Eb", u_), "Sw", "Sbf", ("bk", 3 + u_)], w=["Sw"])
                            G(lambda p=p: nc.gpsimd.tensor_copy(out=Sbf[:, p * 256:(p + 1) * 256], in_=Sw[:, p * 256:(p + 1) * 256]),
                              r=["Sw"], w=["Sbf"])
                            if dirn:
                                VA(lambda p=p, op_=op_: nc.vector.tensor_copy(out=obuf[:, p * 256:(p + 1) * 256], in_=op_),
                                   lambda p=p, op_=op_: nc.scalar.copy(out=obuf[:, p * 256:(p + 1) * 256], in_=op_), r=[("bk", 3 + u_)], w=["obuf"])
                            else:
                                V(lambda p=p, u_=u_, op_=op_, r_=r_: nc.vector.tensor_tensor(out=osum[u_][:], in0=op_, in1=obt[r_][:, p * 256:(p + 1) * 256], op=ALU.add),
                                  r=[("bk", 3 + u_), ("obt", r_)], w=[("osum", u_)])
                                G(lambda u_=u_: nc.gpsimd.memset(ssh[:, 2 * u_:2 * u_ + 2], 0.0), w=[("ssh", u_)])
                                for hb in range(2):
                                    A(lambda u_=u_, hb=hb: nc.scalar.activation(out=junk2[:], in_=osum[u_][:, hb * 128:(hb + 1) * 128], func=AF.Square,
                                                                                scale=128.0 ** -0.5, accum_out=ssh[:, 2 * u_ + hb:2 * u_ + hb + 1]),
                                      r=[("osum", u_), ("ssh", u_)], w=[("ssh", u_), "junk2"])
                                A(lambda u_=u_: nc.scalar.activation(out=rsh[:, 2 * u_:2 * u_ + 2], in_=ssh[:, 2 * u_:2 * u_ + 2], func=AF.Sqrt, bias=EPS, scale=1.0),
                                  r=[("ssh", u_)], w=[("rsh", u_)])
                                V(lambda u_=u_: nc.vector.reciprocal(out=rsh[:, 2 * u_:2 * u_ + 2], in_=rsh[:, 2 * u_:2 * u_ + 2]), r=[("rsh", u_)], w=[("rsh", u_)])
                                G(lambda p=p, u_=u_, r_=r_: nc.gpsimd.tensor_tensor(out=gsb[u_][:], in0=ggb[:, p * 256:(p + 1) * 256], in1=srt[r_][:, p * 256:(p + 1) * 256], op=ALU.mult),
                                  r=["ggb", ("srt", r_)], w=[("gsb", u_)])
                                for hb in range(2):
                                    V(lambda u_=u_, hb=hb: nc.vector.scalar_tensor_tensor(
                                        out=onb[u_][:, hb * 128:(hb + 1) * 128], in0=osum[u_][:, hb * 128:(hb + 1) * 128],
                                        scalar=rsh[:, 2 * u_ + hb:2 * u_ + hb + 1], in1=gsb[u_][:, hb * 128:(hb + 1) * 128], op0=ALU.mult, op1=ALU.mult),
                                      r=[("osum", u_), ("rsh", u_), ("gsb", u_)], w=[("onb", u_)])
                                for hb in range(2):
                                    tp = ps[:, 5, 256 + hb * 128:256 + (hb + 1) * 128]
                                    P(lambda u_=u_, hb=hb, tp=tp: nc.tensor.transpose(out=tp, in_=onb[u_][:, hb * 128:(hb + 1) * 128], identity=ident),
                                      r=[("onb", u_)], w=[("bk", 5)])
                                    VA(lambda p=p, hb=hb, tp=tp, rows=rows: nc.vector.tensor_copy(out=actT[:, 2 * p + hb, rows], in_=tp),
                                       lambda p=p, hb=hb, tp=tp, rows=rows: nc.scalar.copy(out=actT[:, 2 * p + hb, rows], in_=tp),
                                       r=[("bk", 5)], w=[("actT", 2 * p + hb)])
                        if dirn:
                            DM(lambda rows=rows: nc.sync.dma_start(out=ob_d[rows, :], in_=obuf[:]), r=["obuf"], w=[("ob_d", t)])
                S.flush()
                if stop == "B3":
                    return nc

            with contextlib.ExitStack() as ph:
                T = lambda n, s, d=F32: ph.enter_context(nc.sbuf_tensor(uname(n), list(s), d))
                ps = ph.enter_context(nc.psum_tensor("psC", [128, 8, 512], F32))
                ybuf = T("ybuf", [128, NCC, TOK])
                ub = [T("ub%d" % i, [128, TOK], BF16) for i in range(2)]
                ysq = [T("ysq%d" % i, [128, TOK]) for i in range(2)]
                wrow = T("wrow", [32, DC]); crow = T("crow", [4, DC])
                wdw = T("wdw", [128, NCC, 32]); cv = T("cv", [128, NCC, 4])
                mu = T("mu", [128, TOK]); rsv = T("rsv", [128, TOK]); t1 = [T("t1_%d" % i, [128, TOK]) for i in range(2)]
                V(lambda: nc.vector.memset(wrow[:], 0.0), w=["wrow"])
                V(lambda: nc.vector.memset(crow[:], 0.0), w=["crow"])
                DM(lambda: nc.sync.dma_start(out=wrow[0:CONV_W, :], in_=w_dw), w=["wrow"])
                DM(lambda: nc.sync.dma_start(out=crow[0:3, :], in_=cvec), w=["crow"])
                for cch in range(NCC):
                    q = cch % 2
                    P(lambda cch=cch, q=q: nc.tensor.transpose(out=ps[:, 4, q * 64:q * 64 + 32], in_=wrow[:, cch * 128:(cch + 1) * 128], identity=ident[0:32, 0:32]),
                      r=["wrow"], w=[("bk", 4)])
                    V(lambda cch=cch, q=q: nc.vector.tensor_copy(out=wdw[:, cch, :], in_=ps[:, 4, q * 64:q * 64 + 32]), r=[("bk", 4)], w=["wdw"])
                    P(lambda cch=cch, q=q: nc.tensor.transpose(out=ps[:, 4, 256 + q * 64:256 + q * 64 + 4], in_=crow[:, cch * 128:(cch + 1) * 128], identity=ident[0:4, 0:4]),
                      r=["crow"], w=[("bk", 4)])
                    V(lambda cch=cch, q=q: nc.vector.tensor_copy(out=cv[:, cch, :], in_=ps[:, 4, 256 + q * 64:256 + q * 64 + 4]), r=[("bk", 4)], w=["cv"])
                GW = c.GRID_W
                NH2 = TOK // NTB
                for c0 in range(0, NCC, 2):
                    cs = [cc_ for cc_ in (c0, c0 + 1) if cc_ < NCC]
                    for cc_ in cs:
                        DM(lambda cc_=cc_: nc.sync.dma_start(out=ub[cc_ % 2][:], in_=u_d[cc_ * 128:(cc_ + 1) * 128, :]), w=[("ub", cc_ % 2)])
                    for j in [15] + [j for j in range(CONV_W) if j != 15]:
                        for cc_ in cs:
                            uv = ub[cc_ % 2][:].rearrange("p (r t) -> p r t", t=GW)
                            yv = ybuf[:, cc_, :].rearrange("p (r t) -> p r t", t=GW)
                            if j == 15:
                                V(lambda cc_=cc_: nc.vector.tensor_scalar(out=ybuf[:, cc_, :], in0=ub[cc_ % 2][:], scalar1=wdw[:, cc_, 15:16],
                                                                          scalar2=cv[:, cc_, 0:1], op0=ALU.mult, op1=ALU.add),
                                  r=[("ub", cc_ % 2), "wdw", "cv"], w=[("y", cc_)])
                            else:
                                d_ = j - 15
                                lo_o, hi_o = max(0, -d_), GW - max(0, d_)
                                lo_i, hi_i = max(0, d_), GW - max(0, -d_)
                                V(lambda cc_=cc_, j=j, uv=uv, yv=yv, lo_o=lo_o, hi_o=hi_o, lo_i=lo_i, hi_i=hi_i: nc.vector.scalar_tensor_tensor(
                                    out=yv[:, :, lo_o:hi_o], in0=uv[:, :, lo_i:hi_i], scalar=wdw[:, cc_, j:j + 1], in1=yv[:, :, lo_o:hi_o],
                                    op0=ALU.mult, op1=ALU.add), r=[("ub", cc_ % 2), ("y", cc_)], w=[("y", cc_)])
                    for cc_ in cs:
                        A(lambda cc_=cc_: nc.scalar.activation(out=ysq[cc_ % 2][:], in_=ybuf[:, cc_, :], func=AF.Square), r=[("y", cc_)], w=[("ysq", cc_ % 2)])
                        for hf in range(NH2):
                            P(lambda cc_=cc_, hf=hf: nc.tensor.matmul(out=ps[:, hf, 0:NTB], lhsT=CM("ones"), rhs=ybuf[:, cc_, hf * NTB:(hf + 1) * NTB],
                                                                     start=(cc_ == 0), stop=(cc_ == NCC - 1)), r=[("y", cc_)], w=[("s1", hf)])
                            P(lambda cc_=cc_, hf=hf: nc.tensor.matmul(out=ps[:, 2 + hf, 0:NTB], lhsT=CM("ones"), rhs=ysq[cc_ % 2][:, hf * NTB:(hf + 1) * NTB],
                                                                     start=(cc_ == 0), stop=(cc_ == NCC - 1)), r=[("ysq", cc_ % 2)], w=[("s2", hf)])
                for hf in range(NH2):
                    sl = slice(hf * NTB, (hf + 1) * NTB)
                    A(lambda hf=hf, sl=sl: nc.scalar.mul(out=mu[:, sl], in_=ps[:, hf, 0:NTB], mul=1.0 / DC), r=[("s1", hf)], w=[("mu", hf)])
                    V(lambda hf=hf, sl=sl: nc.vector.tensor_tensor(out=rsv[:, sl], in0=mu[:, sl], in1=mu[:, sl], op=ALU.mult), r=[("mu", hf)], w=[("rsv", hf)])
                    V(lambda hf=hf, sl=sl: nc.vector.scalar_tensor_tensor(out=rsv[:, sl], in0=ps[:, 2 + hf, 0:NTB], scalar=1.0 / DC, in1=rsv[:, sl],
                                                                          op0=ALU.mult, op1=ALU.subtract), r=[("s2", hf), ("rsv", hf)], w=[("rsv", hf)])
                    A(lambda hf=hf, sl=sl: nc.scalar.activation(out=rsv[:, sl], in_=rsv[:, sl], func=AF.Sqrt, bias=EPS, scale=1.0), r=[("rsv", hf)], w=[("rsv", hf)])
                    V(lambda hf=hf, sl=sl: nc.vector.reciprocal(out=rsv[:, sl], in_=rsv[:, sl]), r=[("rsv", hf)], w=[("rsv", hf)])
                rk = [("rsv", hf) for hf in range(NH2)]; mk = [("mu", hf) for hf in range(NH2)]
                for cc_ in range(NCC):
                    i_ = cc_ % 2
                    G(lambda cc_=cc_, i_=i_: nc.gpsimd.tensor_tensor(out=t1[i_][:], in0=ybuf[:, cc_, :], in1=mu[:], op=ALU.subtract), r=[("y", cc_)] + mk, w=[("t1", i_)])
                    V(lambda i_=i_: nc.vector.tensor_tensor(out=t1[i_][:], in0=t1[i_][:], in1=rsv[:], op=ALU.mult), r=[("t1", i_)] + rk, w=[("t1", i_)])
                    A(lambda cc_=cc_, i_=i_: nc.scalar.activation(out=actT[:, c.NH + cc_, :], in_=t1[i_][:], func=AF.Silu, scale=cv[:, cc_, 1:2], bias=cv[:, cc_, 2:3]),
                      r=[("t1", i_), "cv"], w=[("actT", c.NH + cc_)])
                S.flush()
                if stop == "B4":
                    return nc

            with contextlib.ExitStack() as ph:
                T = lambda n, s, d=F32: ph.enter_context(nc.sbuf_tensor(uname(n), list(s), d))
                ps = ph.enter_context(nc.psum_tensor("psO", [128, 8, 512], F32))
                Tw = {"st": [T("stO%d" % i, [128, KP, 256]) for i in range(3)], "wb": [T("wbO%d" % i, [128, KD, 256], BF16) for i in range(2)],
                      "wi": [0], "si": [0]}
                gam = T("gam", [128, D])
                xs_ = [T("xsl%d" % i, [128, 256]) for i in range(3)]; tm_ = [T("tml%d" % i, [128, 256]) for i in range(3)]
                DM(lambda: nc.sync.dma_start(out=gam[:], in_=mod_d[0, 2 * D:3 * D].partition_broadcast(128)), w=["gam"])
                n_ = 0
                for c0 in range(0, D, 256):
                    wb, wkey = load_cast_w(Tw, lambda k0, k1, c0=c0: w_out[k0 * 128:k1 * 128, c0:c0 + 256], KD, 256, "O")
                    for t in range(NT):
                        bank = n_ % 6; i_ = n_ % 3; n_ += 1
                        rows = slice(t * 128, (t + 1) * 128)
                        DM(lambda i_=i_, rows=rows, c0=c0: nc.sync.dma_start(out=xs_[i_][:], in_=x_own[rows, c0:c0 + 256]), w=[("xsl", i_)])
                        for k in range(KD):
                            P(lambda k=k, rows=rows, wb=wb, bank=bank: nc.tensor.matmul(out=ps[:, bank, 0:256], lhsT=actT[:, k, rows], rhs=wb[:, k, :],
                                                                                       start=(k == 0), stop=(k == KD - 1)), r=[wkey], w=[("pb", bank)])
                        V(lambda i_=i_, bank=bank, c0=c0: nc.vector.tensor_tensor(out=tm_[i_][:], in0=ps[:, bank, 0:256], in1=gam[:, c0:c0 + 256], op=ALU.mult),
                          r=[("pb", bank), "gam"], w=[("tml", i_)])
                        G(lambda i_=i_: nc.gpsimd.tensor_tensor(out=tm_[i_][:], in0=tm_[i_][:], in1=xs_[i_][:], op=ALU.add),
                          r=[("tml", i_), ("xsl", i_)], w=[("tml", i_)])
                        DM(lambda i_=i_, rows=rows, c0=c0: nc.sync.dma_start(out=xlat_d[rows, c0:c0 + 256], in_=tm_[i_][:]), r=[("tml", i_)], w=["xlat_d"])
                S.flush()
                if stop == "B5":
                    return nc

        gla_scope.close()
        moe = contextlib.ExitStack()
        with moe:
            MT = lambda n, s, d=F32: moe.enter_context(nc.sbuf_tensor(uname(n), list(s), d))
            Wr_all = MT("Wr_all", [128, NT, E]); mask_all = MT("mask_all", [128, NT, E])
            idxg = MT("idxg", [128, NB * E], I32); idxs = MT("idxs", [128, NB * E], I32); wsl = MT("wsl", [128, NB * E])
            with contextlib.ExitStack() as shs:
                h2T = shs.enter_context(nc.sbuf_tensor("s_h2T", [128, KD, TOK], BF16))
                with contextlib.ExitStack() as ph:
                    T = lambda n, s, d=F32: ph.enter_context(nc.sbuf_tensor(uname(n), list(s), d))
                    ps = ph.enter_context(nc.psum_tensor("psR", [128, 8, 512], F32))
                    Tn = norm_bufs(T, with_xb=True)
                    h2f = T("h2f", [128, KD, 128]); wrt = T("wrt", [128, KD, E]); brb = T("brb", [128, E])
                    sc = [T("sc%d" % i, [128, E]) for i in range(2)]; sel = [T("sel%d" % i, [128, E]) for i in range(2)]
                    sel2 = [T("sel2%d" % i, [128, E]) for i in range(2)]
                    sm = [T("sm%d" % i, [128, 64]) for i in range(2)]
                    DM(lambda: nc.sync.dma_start(out=wrt[:], in_=w_rt.rearrange("(k p) n -> p k n", p=128)), w=["wrt"])
                    DM(lambda: nc.sync.dma_start(out=brb[:], in_=b_rt.partition_broadcast(128)), w=["brb"])
                    lvl6 = int(os.environ.get('KDBG_B6', '999')); rc = [0]

                    def VR(fn, r=(), w=()):
                        rc[0] += 1
                        if rc[0] <= lvl6: V(fn, r, w)

                    def AR(fn, r=(), w=()):
                        rc[0] += 1
                        if rc[0] <= lvl6: A(fn, r, w)

                    for t in range(NT):
                        rc[0] = 0
                        rows = slice(t * 128, (t + 1) * 128); i_ = t % 2
                        n6 = int(os.environ.get('KDBG_B6N', '9'))
                        norm_tile(Tn, ps, xlat_d[rows, :], Gf, shf, lambda k, rows=rows: (h2T[:, k, rows], ("h2T", k)), t,
                                  xs2_dst=(xs2_d[rows, :] if n6 >= 1 else None), h2f=((lambda k: (h2f[:, k, :], ("h2f", k))) if n6 >= 2 else None), tpbanks=(4, 6))
                        for k in range(KD if n6 >= 3 else 0):
                            P(lambda k=k: nc.tensor.matmul(out=ps[:, 5, 0:E], lhsT=h2f[:, k, :], rhs=wrt[:, k, :], start=(k == 0), stop=(k == KD - 1)),
                              r=[("h2f", k), "wrt"], w=[("bk", 5)])
                        m = sm[i_]
                        AR(lambda i_=i_: nc.scalar.activation(out=sc[i_][:], in_=ps[:, 5, 0:E], func=AF.Sigmoid), r=[("bk", 5)], w=[("sc", i_)])
                        VR(lambda i_=i_: nc.vector.tensor_tensor(out=sel[i_][:], in0=sc[i_][:], in1=brb[:], op=ALU.add), r=[("sc", i_), "brb"], w=[("sel", i_)])
                        k1 = ("rt", i_)
                        VR(lambda i_=i_, m=m: nc.vector.tensor_reduce(out=m[:, 0:8], in_=sel[i_][:].rearrange("p (g e) -> p g e", e=c.EPG), axis=AX.X, op=ALU.max),
                          r=[("sel", i_)], w=[k1])
                        for g in range(NGRP):
                            gs_ = slice(g * c.EPG, (g + 1) * c.EPG)
                            VR(lambda i_=i_, m=m, g=g, gs_=gs_: nc.vector.tensor_scalar(out=sel2[i_][:, gs_], in0=sel[i_][:, gs_], scalar1=m[:, g:g + 1], scalar2=-1e9,
                                                                                       op0=ALU.is_equal, op1=ALU.mult), r=[k1, ("sel", i_)], w=[("sel2", i_)])
                        VR(lambda i_=i_: nc.vector.tensor_tensor(out=sel2[i_][:], in0=sel2[i_][:], in1=sel[i_][:], op=ALU.add), r=[("sel2", i_), ("sel", i_)], w=[("sel2", i_)])
                        VR(lambda i_=i_, m=m: nc.vector.tensor_reduce(out=m[:, 8:16], in_=sel2[i_][:].rearrange("p (g e) -> p g e", e=c.EPG), axis=AX.X, op=ALU.max),
                          r=[("sel2", i_)], w=[k1])
                        VR(lambda m=m: nc.vector.tensor_tensor(out=m[:, 8:16], in0=m[:, 8:16], in1=m[:, 0:8], op=ALU.add), r=[k1], w=[k1])
                        VR(lambda m=m: nc.vector.max(out=m[:, 16:24], in_=m[:, 8:16]), r=[k1], w=[k1])
                        VR(lambda m=m: nc.vector.tensor_scalar(out=m[:, 24:32], in0=m[:, 8:16], scalar1=m[:, 16 + TOPG - 1:16 + TOPG], scalar2=None, op0=ALU.is_ge), r=[k1], w=[k1])
                        VR(lambda m=m: nc.vector.tensor_scalar(out=m[:, 32:40], in0=m[:, 24:32], scalar1=-1.0, scalar2=1e9, op0=ALU.add, op1=ALU.mult), r=[k1], w=[k1])
                        for g in range(NGRP):
                            gs_ = slice(g * c.EPG, (g + 1) * c.EPG)
                            VR(lambda i_=i_, m=m, g=g, gs_=gs_: nc.vector.tensor_scalar(out=sel2[i_][:, gs_], in0=sel[i_][:, gs_], scalar1=m[:, 32 + g:33 + g], scalar2=None,
                                                                                       op0=ALU.add), r=[k1, ("sel", i_), ("sel2", i_)], w=[("sel2", i_)])
                        VR(lambda i_=i_, m=m: nc.vector.max(out=m[:, 40:48], in_=sel2[i_][:]), r=[("sel2", i_), k1], w=[k1])
                        VR(lambda i_=i_, m=m, t=t: nc.vector.tensor_scalar(out=mask_all[:, t, :], in0=sel2[i_][:], scalar1=m[:, 40 + TOPK - 1:40 + TOPK], scalar2=None, op0=ALU.is_ge),
                          r=[("sel2", i_), k1], w=[("mask", t)])
                        VR(lambda i_=i_, t=t: nc.vector.tensor_tensor(out=sel[i_][:], in0=sc[i_][:], in1=mask_all[:, t, :], op=ALU.mult), r=[("sc", i_), ("mask", t), ("sel", i_), ("sel2", i_)], w=[("sel", i_)])
                        VR(lambda i_=i_, m=m: nc.vector.tensor_reduce(out=m[:, 48:49], in_=sel[i_][:], axis=AX.X, op=ALU.add), r=[("sel", i_), k1], w=[k1])
                        VR(lambda m=m: nc.vector.reciprocal(out=m[:, 49:50], in_=m[:, 48:49]), r=[k1], w=[k1])
                        VR(lambda i_=i_, m=m, t=t: nc.vector.tensor_scalar(out=Wr_all[:, t, :], in0=sel[i_][:], scalar1=m[:, 49:50], scalar2=RSCALE, op0=ALU.mult, op1=ALU.mult),
                          r=[("sel", i_), k1], w=[("Wr", t)])
                    S.flush()
                    if stop == "B6":
                        return nc

                with contextlib.ExitStack() as ph:
                    T = lambda n, s, d=F32: ph.enter_context(nc.sbuf_tensor(uname(n), list(s), d))
                    ps = ph.enter_context(nc.psum_tensor("psI", [128, 8, 512], F32))
                    pos_all = T("pos_all", [128, NT, E]); vals = T("vals", [128, NT, E, 2])
                    oh = [T("oh%d" % i, [128, C]) for i in range(4)]
                    tab = T("tab", [128, NB, 2 * E]); eq0 = T("eq0", [128, NB, E]); tf = T("tf", [128, NB, E]); tg = T("tg", [128, NB, E])
                    for j in range(NT):
                        for i in range(j + 1):
                            P(lambda i=i, j=j: nc.tensor.matmul(out=ps[:, 4 + j % 2, 0:E], lhsT=(CM("ustr") if i == j else CM("ones")), rhs=mask_all[:, i, :],
                                                               start=(i == 0), stop=(i == j)), w=[("pp", j % 2)])
                        V(lambda j=j: nc.vector.tensor_copy(out=pos_all[:, j, :], in_=ps[:, 4 + j % 2, 0:E]), r=[("pp", j % 2)], w=["pos"])
                    for i in range(NT):
                        V(lambda i=i: nc.vector.tensor_scalar(out=vals[:, i, :, 0], in0=mask_all[:, i, :], scalar1=0.0, scalar2=CM("tokid1")[:, i:i + 1],
                                                              op0=ALU.mult, op1=ALU.add), w=["vals"])
                        G(lambda i=i: nc.gpsimd.tensor_copy(out=vals[:, i, :, 1], in_=Wr_all[:, i, :]), w=["vals"])
                    n_ = 0
                    for e in range(E):
                        for i in range(NT):
                            o_ = n_ % 4; n_ += 1
                            V(lambda e=e, i=i, o_=o_: nc.vector.tensor_scalar(out=oh[o_][:], in0=CM("iotaC"), scalar1=pos_all[:, i, e:e + 1], scalar2=mask_all[:, i, e:e + 1],
                                                                              op0=ALU.is_equal, op1=ALU.mult), r=["pos"], w=[("oh", o_)])
                            for b in range(NB):
                                P(lambda e=e, i=i, o_=o_, b=b: nc.tensor.matmul(out=ps[:, b, 2 * e:2 * e + 2], lhsT=oh[o_][:, b * 128:(b + 1) * 128], rhs=vals[:, i, e, :],
                                                                               start=(i == 0), stop=(i == NT - 1)), r=[("oh", o_), "vals"], w=[("ib", b)])
                    for b in range(NB):
                        V(lambda b=b: nc.vector.tensor_copy(out=tab[:, b, :], in_=ps[:, b, 0:2 * E]), r=[("ib", b)], w=["tab"])
                    tv = tab[:].rearrange("p b (e two) -> p b e two", two=2)
                    fl = lambda t_: t_[:].rearrange("p b e -> p (b e)")
                    V(lambda: nc.vector.tensor_copy(out=wsl[:], in_=tv[:, :, :, 1].rearrange("p b e -> p (b e)")), r=["tab"], w=["wsl"])
                    V(lambda: nc.vector.tensor_scalar(out=eq0[:], in0=tv[:, :, :, 0], scalar1=0.0, scalar2=None, op0=ALU.is_equal), r=["tab"], w=["eq0"])
                    V(lambda: nc.vector.scalar_tensor_tensor(out=tf[:], in0=eq0[:], scalar=float(TOK + 1), in1=tv[:, :, :, 0], op0=ALU.mult, op1=ALU.add), r=["eq0", "tab"], w=["tf"])
                    V(lambda: nc.vector.tensor_scalar(out=tf[:], in0=tf[:], scalar1=-1.0, scalar2=None, op0=ALU.add), r=["tf"], w=["tf"])
                    V(lambda: nc.vector.tensor_copy(out=idxg[:], in_=fl(tf)), r=["tf"], w=["idxg"])
                    V(lambda: nc.vector.tensor_scalar(out=tg[:], in0=eq0[:], scalar1=CM("trash")[:, 0:1], scalar2=None, op0=ALU.mult), r=["eq0"], w=["tg"])
                    V(lambda: nc.vector.tensor_tensor(out=tg[:], in0=tg[:], in1=eq0[:], op=ALU.add), r=["tg", "eq0"], w=["tg"])
                    V(lambda: nc.vector.tensor_tensor(out=tg[:], in0=tg[:], in1=tv[:, :, :, 0], op=ALU.add), r=["tg", "tab"], w=["tg"])
                    V(lambda: nc.vector.tensor_scalar(out=tg[:], in0=tg[:], scalar1=-1.0, scalar2=None, op0=ALU.add), r=["tg"], w=["tg"])
                    V(lambda: nc.vector.tensor_copy(out=idxs[:], in_=fl(tg)), r=["tg"], w=["idxs"])
                    if dbg:
                        d1 = nc.dram_tensor("dbg_idxg", [128, NB * E], I32, kind="ExternalOutput").ap()
                        d2 = nc.dram_tensor("dbg_idxs", [128, NB * E], I32, kind="ExternalOutput").ap()
                        d3 = nc.dram_tensor("dbg_wsl", [128, NB * E], F32, kind="ExternalOutput").ap()
                        d4 = nc.dram_tensor("dbg_wr", [128, NT * E], F32, kind="ExternalOutput").ap()
                        d5 = nc.dram_tensor("dbg_pos", [128, NT * E], F32, kind="ExternalOutput").ap()
                        DM(lambda: nc.sync.dma_start(out=d1, in_=idxg[:]), r=["idxg"])
                        DM(lambda: nc.sync.dma_start(out=d2, in_=idxs[:]), r=["idxs"])
                        DM(lambda: nc.sync.dma_start(out=d3, in_=wsl[:]), r=["wsl"])
                        DM(lambda: nc.sync.dma_start(out=d4, in_=Wr_all[:].rearrange("p t e -> p (t e)")))
                        DM(lambda: nc.sync.dma_start(out=d5, in_=pos_all[:].rearrange("p t e -> p (t e)")), r=["pos"])
                    S.flush()
                    if stop == "B7":
                        return nc

                with contextlib.ExitStack() as ph:
                    T = lambda n, s, d=F32: ph.enter_context(nc.sbuf_tensor(uname(n), list(s), d))
                    ps = ph.enter_context(nc.psum_tensor("psS", [128, 8, 512], F32))
                    Tw = {"st": [T("stS%d" % i, [128, KP, 256]) for i in range(3)], "wb": [T("wbS%d" % i, [128, KD, 256], BF16) for i in range(3)],
                          "wi": [0], "si": [0]}
                    HsT = T("HsT", [128, c.NSB, TOK], BF16); tsl = [T("tsl%d" % i, [128, NTB]) for i in range(2)]
                    ysb = [T("ysb%d" % i, [128, 256]) for i in range(3)]; zt = T("zt", [128, c.DQ])
                    V(lambda: nc.vector.memset(zt[:], 0.0), w=["zt"])
                    for q in range(c.NQ):
                        DM(lambda q=q: nc.sync.dma_start(out=y_q[q][TOK:TOK + 128, :], in_=zt[:]), r=["zt"], w=[("yq", q)])
                    n_ = 0
                    for hb in range(c.NSB):
                        wg_, kg = load_cast_w(Tw, lambda k0, k1, hb=hb: w_sg[k0 * 128:k1 * 128, hb * 128:(hb + 1) * 128], KD, 128, "S")
                        wu_, ku = load_cast_w(Tw, lambda k0, k1, hb=hb: w_su[k0 * 128:k1 * 128, hb * 128:(hb + 1) * 128], KD, 128, "S")
                        for tb in range(TOK // NTB):
                            bg_ = (2 * n_) % 6; bu_ = (2 * n_ + 1) % 6; i_ = n_ % 2; n_ += 1
                            for (wb_, wk_, bank) in ((wg_, kg, bg_), (wu_, ku, bu_)):
                                for k in range(KD):
                                    P(lambda k=k, wb_=wb_, bank=bank, tb=tb: nc.tensor.matmul(out=ps[:, bank, 0:NTB], lhsT=wb_[:, k, 0:128], rhs=h2T[:, k, tb * NTB:(tb + 1) * NTB],
                                                                                             start=(k == 0), stop=(k == KD - 1)), r=[wk_], w=[("pb", bank)])
                            A(lambda i_=i_, bg_=bg_: nc.scalar.activation(out=tsl[i_][:], in_=ps[:, bg_, 0:NTB], func=AF.Silu), r=[("pb", bg_)], w=[("tsl", i_)])
                            V(lambda i_=i_, bu_=bu_, hb=hb, tb=tb: nc.vector.tensor_tensor(out=HsT[:, hb, tb * NTB:(tb + 1) * NTB], in0=tsl[i_][:], in1=ps[:, bu_, 0:NTB], op=ALU.mult),
                              r=[("tsl", i_), ("pb", bu_)], w=["HsT"])
                    n_ = 0
                    for c0 in range(0, D, 256):
                        wb, wkey = load_cast_w(Tw, lambda k0, k1, c0=c0: w_sd[k0 * 128:k1 * 128, c0:c0 + 256], c.NSB, 256, "S")
                        q = c0 // c.DQ; qo = c0 % c.DQ
                        for t in range(NT):
                            bank = n_ % 6; i_ = n_ % 3; n_ += 1
                            rows = slice(t * 128, (t + 1) * 128)
                            for kk in range(c.NSB):
                                P(lambda kk=kk, wb=wb, bank=bank, rows=rows: nc.tensor.matmul(out=ps[:, bank, 0:256], lhsT=HsT[:, kk, rows], rhs=wb[:, kk, :],
                                                                                             start=(kk == 0), stop=(kk == c.NSB - 1)), r=[wkey, "HsT"], w=[("pb", bank)])
                            VA(lambda i_=i_, bank=bank: nc.vector.tensor_copy(out=ysb[i_][:], in_=ps[:, bank, 0:256]),
                               lambda i_=i_, bank=bank: nc.scalar.copy(out=ysb[i_][:], in_=ps[:, bank, 0:256]), r=[("pb", bank)], w=[("ysb", i_)])
                            DM(lambda i_=i_, q=q, qo=qo, rows=rows: nc.sync.dma_start(out=y_q[q][rows, qo:qo + 256], in_=ysb[i_][:]), r=[("ysb", i_)], w=[("yq", q)])
                    S.flush()
                    if stop == "B8":
                        return nc

            with contextlib.ExitStack() as ph:
                T = lambda n, s, d=F32: ph.enter_context(nc.sbuf_tensor(uname(n), list(s), d))
                ps = ph.enter_context(nc.psum_tensor("psE", [128, 6, 512], F32))
                psT = ph.enter_context(nc.psum_tensor("psT", [128, 2, 1024], BF16))
                NXE = NB + (1 if C < 512 else 0)
                Xe = [T("Xe%d" % i, [128, D], BF16) for i in range(NXE)]
                XeT = T("XeT", [128, KD, C], BF16)
                HT = [T("HT%d" % i, [128, c.NHB, C], BF16) for i in range(2)]
                KH = max(1, KD // 2)
                stE = [T("stE%d" % i, [128, KH, 128]) for i in range(4)]
                wgb = [T("wgb%d" % i, [128, KD, 128], BF16) for i in range(3)]
                stD = [T("stD%d" % i, [128, c.NHB, c.DW]) for i in range(2)]
                wdb = [T("wdb%d" % i, [128, c.NHB, c.DW], BF16) for i in range(2)]
                Ost = [T("Ost%d" % i, [128, c.DQ]) for i in range(NB)]
                tsl = [T("tse%d" % i, [128, C]) for i in range(2)]
                for i in range(NXE):
                    V(lambda i=i: nc.vector.memset(Xe[i][:], 0.0), w=[("Xe", i)])
                sn = 0; gn = 0; dn = 0; on = 0; bn = 0; tn = 0
                xn = [0]
                reg_g = nc.gpsimd.to_reg(TOK - 1); reg_s = nc.gpsimd.to_reg(TOK + 127)

                def issue_gathers(e):
                    xr_ = []
                    for b in range(NB):
                        r_ = xn[0] % NXE; xn[0] += 1; xr_.append(r_)
                        col = b * E + e
                        GD(lambda r_=r_, col=col: nc.gpsimd.indirect_dma_start(
                            out=Xe[r_][:], out_offset=None, in_=xs2_d, in_offset=bass.IndirectOffsetOnAxis(ap=idxg[:, col:col + 1], axis=0),
                            bounds_check=reg_g, oob_is_err=False), w=[("Xe", r_)])
                    return xr_

                pend = {}
                cnt_ = {"sn": 0, "gn": 0}

                def prefetch_gu(e, hb):
                    out_ = []
                    for wsrc in (w_eg, w_eu):
                        g_ = cnt_["gn"] % 3; cnt_["gn"] += 1
                        base = (e * c.NHB + hb) * 128
                        for k0 in range(0, KD, KH):
                            s_ = cnt_["sn"] % 4; cnt_["sn"] += 1
                            DM(lambda wsrc=wsrc, base=base, k0=k0, s_=s_: nc.sync.dma_start(
                                out=stE[s_][:], in_=wsrc[base:base + 128, k0 * 128:(k0 + KH) * 128].rearrange("p (k n) -> p k n", n=128)), w=[("stE", s_)])
                            V(lambda g_=g_, k0=k0, s_=s_: nc.vector.tensor_copy(out=wgb[g_][:, k0:k0 + KH, :], in_=stE[s_][:]), r=[("stE", s_)], w=[("wgb", g_)])
                        out_.append(g_)
                    pend[(e, hb)] = out_

                prefetch_gu(0, 0)
                xr_next = issue_gathers(0)
                for e in range(E):
                    xr = xr_next
                    for k in range(KD):
                        tb_ = tn % 2; tn += 1
                        for b in range(NB):
                            P(lambda k=k, b=b, tb_=tb_, r_=xr[b]: nc.tensor.transpose(out=psT[:, tb_, b * 128:(b + 1) * 128], in_=Xe[r_][:, k * 128:(k + 1) * 128], identity=idb[:]),
                              r=[("Xe", xr[b])], w=[("pT", tb_)])
                        VA(lambda k=k, tb_=tb_: nc.vector.tensor_scalar(out=XeT[:, k, :], in0=psT[:, tb_, 0:C], scalar1=Gf[:, k:k + 1], scalar2=shf[:, k:k + 1], op0=ALU.mult, op1=ALU.add),
                           lambda k=k, tb_=tb_: nc.scalar.activation(out=XeT[:, k, :], in_=psT[:, tb_, 0:C], func=AF.Identity, scale=Gf[:, k:k + 1], bias=shf[:, k:k + 1]),
                           r=[("pT", tb_)], w=[("XeT", k)])
                    if e + 1 < E:
                        xr_next = issue_gathers(e + 1)
                    hbuf = HT[e % 2]
                    for hb in range(c.NHB):
                        gs_ = pend.pop((e, hb))
                        banks = []
                        for g_ in gs_:
                            bank = bn % 4; bn += 1; banks.append(bank)
                            for k in range(KD):
                                P(lambda k=k, g_=g_, bank=bank: nc.tensor.matmul(out=ps[:, bank, 0:C], lhsT=wgb[g_][:, k, :], rhs=XeT[:, k, :], start=(k == 0), stop=(k == KD - 1)),
                                  r=[("wgb", g_), ("XeT", k)], w=[("pb", bank)])
                        if hb + 1 < c.NHB:
                            prefetch_gu(e, hb + 1)
                        i_ = (e * c.NHB + hb) % 2
                        A(lambda i_=i_, bank=banks[0]: nc.scalar.activation(out=tsl[i_][:], in_=ps[:, bank, 0:C], func=AF.Silu), r=[("pb", banks[0])], w=[("tse", i_)])
                        V(lambda i_=i_, hb=hb, hbuf=hbuf, bank=banks[1]: nc.vector.tensor_tensor(out=hbuf[:, hb, :], in0=tsl[i_][:], in1=ps[:, bank, 0:C], op=ALU.mult),
                          r=[("tse", i_), ("pb", banks[1])], w=[("HT", e % 2)])
                    if e + 1 < E:
                        prefetch_gu(e + 1, 0)
                    DW = c.DW
                    for q in range(c.NQ):
                        for sub in range(c.DQ // DW):
                            d_ = dn % 2; dn += 1
                            c0 = q * c.DQ + sub * DW
                            DM(lambda e=e, c0=c0, d_=d_: nc.sync.dma_start(out=stD[d_][:], in_=w_ed[e * c.DE:(e + 1) * c.DE, c0:c0 + DW].rearrange("(k p) n -> p k n", p=128)),
                               w=[("stD", d_)])
                            VA(lambda d_=d_: nc.vector.tensor_copy(out=wdb[d_][:], in_=stD[d_][:]),
                               lambda d_=d_: nc.scalar.copy(out=wdb[d_][:], in_=stD[d_][:]), r=[("stD", d_)], w=[("wdb", d_)])
                            for b in range(NB):
                                bank = 4 + (on % 2); on += 1
                                col = b * E + e
                                for kk in range(c.NHB):
                                    P(lambda kk=kk, b=b, d_=d_, bank=bank, hbuf=hbuf: nc.tensor.matmul(out=ps[:, bank, 0:DW], lhsT=hbuf[:, kk, b * 128:(b + 1) * 128], rhs=wdb[d_][:, kk, :],
                                                                                                  start=(kk == 0), stop=(kk == c.NHB - 1)), r=[("wdb", d_), ("HT", e % 2)], w=[("pd", bank)])
                                osl = slice(sub * DW, (sub + 1) * DW)
                                VA(lambda b=b, bank=bank, col=col, osl=osl: nc.vector.tensor_scalar(out=Ost[b][:, osl], in0=ps[:, bank, 0:DW], scalar1=wsl[:, col:col + 1], scalar2=None, op0=ALU.mult),
                                   lambda b=b, bank=bank, col=col, osl=osl: nc.scalar.activation(out=Ost[b][:, osl], in_=ps[:, bank, 0:DW], func=AF.Copy, scale=wsl[:, col:col + 1]),
                                   r=[("pd", bank)], w=[("Ost", b)])
                        for b in range(NB):
                            col = b * E + e
                            GD(lambda b=b, q=q, col=col: nc.gpsimd.indirect_dma_start(
                                out=y_q[q], out_offset=bass.IndirectOffsetOnAxis(ap=idxs[:, col:col + 1], axis=0), in_=Ost[b][:], in_offset=None,
                                bounds_check=reg_s, oob_is_err=True, compute_op=ALU.add), r=[("Ost", b)], w=[("yq", q)])
                S.flush()
                if stop == "B9":
                    return nc

            with contextlib.ExitStack() as ph:
                T = lambda n, s, d=F32: ph.enter_context(nc.sbuf_tensor(uname(n), list(s), d))
                xl = [T("xl%d" % i, [128, D]) for i in range(2)]; yt = [T("yt%d" % i, [128, D]) for i in range(2)]
                gaf = T("gaf", [128, D]); gfin = T("gfin", [128, D]); junk = T("junkF", [128, D], BF16)
                ss = T("ssF", [128, 2]); rs = T("rsF", [128, 2])
                DM(lambda: nc.sync.dma_start(out=gaf[:], in_=mod_d[0, 5 * D:6 * D].partition_broadcast(128)), w=["gaf"])
                DM(lambda: nc.sync.dma_start(out=gfin[:], in_=gvecs[2].partition_broadcast(128)), w=["gfin"])
                for t in range(NT):
                    i_ = t % 2; rows = slice(t * 128, (t + 1) * 128)
                    DM(lambda i_=i_, rows=rows: nc.sync.dma_start(out=xl[i_][:], in_=xlat_d[rows, :]), w=[("xl", i_)])
                    for q in range(c.NQ):
                        DM(lambda i_=i_, rows=rows, q=q: nc.sync.dma_start(out=yt[i_][:, q * c.DQ:(q + 1) * c.DQ], in_=y_q[q][rows, :]), w=[("yt", i_)])
                    V(lambda i_=i_: nc.vector.tensor_tensor(out=yt[i_][:], in0=yt[i_][:], in1=gaf[:], op=ALU.mult), r=[("yt", i_), "gaf"], w=[("yt", i_)])
                    G(lambda i_=i_: nc.gpsimd.tensor_tensor(out=yt[i_][:], in0=yt[i_][:], in1=xl[i_][:], op=ALU.add), r=[("yt", i_), ("xl", i_)], w=[("yt", i_)])
                    G(lambda i_=i_: nc.gpsimd.memset(ss[:, i_:i_ + 1], 0.0), w=[("ssF", i_)])
                    A(lambda i_=i_: nc.scalar.activation(out=junk[:], in_=yt[i_][:], func=AF.Square, scale=float(D) ** -0.5, accum_out=ss[:, i_:i_ + 1]),
                      r=[("yt", i_), ("ssF", i_)], w=[("ssF", i_), "junkF"])
                    A(lambda i_=i_: nc.scalar.activation(out=rs[:, i_:i_ + 1], in_=ss[:, i_:i_ + 1], func=AF.Sqrt, bias=EPS, scale=1.0), r=[("ssF", i_)], w=[("rsF", i_)])
                    V(lambda i_=i_: nc.vector.reciprocal(out=rs[:, i_:i_ + 1], in_=rs[:, i_:i_ + 1]), r=[("rsF", i_)], w=[("rsF", i_)])
                    V(lambda i_=i_: nc.vector.scalar_tensor_tensor(out=xl[i_][:], in0=yt[i_][:], scalar=rs[:, i_:i_ + 1], in1=gfin[:], op0=ALU.mult, op1=ALU.mult),
                      r=[("yt", i_), ("rsF", i_), "gfin", ("xl", i_)], w=[("xl", i_)])
                    DM(lambda i_=i_, rows=rows: nc.sync.dma_start(out=out[rows, :], in_=xl[i_][:]), r=[("xl", i_)], w=["out"])
                S.flush()
    return nc


def _prep_inputs(c, inp):
    f = lambda a: np.ascontiguousarray(a, dtype=np.float32)
    D, TOK, KD = c.D, c.TOK, c.KD
    x = f(inp["x"])[0]; ctx = f(inp["ctx"])[0]
    w_in = f(inp["w_in"])[0]
    DK, DG, DC = c.DK, c.DG, c.DC
    oQ, oK, oV, oR = 0, DK, 2 * DK, 2 * DK + DG
    oGF = oR + DG; oGB = oGF + 16; oCA = oGB + 16; oCB = oCA + DC
    wa = w_in[:, oCA:oCA + DC].reshape(D, c.NCC, 128); wb_ = w_in[:, oCB:oCB + DC].reshape(D, c.NCC, 128)
    w_ab = np.ascontiguousarray(np.concatenate([wa, wb_], axis=2).reshape(D, 2 * DC))
    wgu = f(inp["w_gate_up"])[0]; bgu = f(inp["b_gate_up"])[0]
    wgf, wgb = w_in[:, oGF:oGF + 16], w_in[:, oGB:oGB + 16]
    weg = f(inp["w_e_gate"])[0]; weu = f(inp["w_e_up"])[0]

    def relay(w):
        return np.ascontiguousarray(w.reshape(c.E, KD, 128, c.NHB, 128).transpose(0, 3, 2, 1, 4).reshape(c.E * c.NHB * 128, KD * 128))
    common = {
        "ctx2": np.ascontiguousarray(np.concatenate([ctx, ctx[::-1]], 0)),
        "cc": np.ascontiguousarray(np.stack([f(inp["c"])[0], f(inp["c_ctx"])], 0)),
        "w_ada": f(inp["w_ada"])[0], "b_ada": f(inp["b_ada"])[0],
        "gvecs": np.ascontiguousarray(np.stack([f(inp["g_norm_mix"])[0], f(inp["g_norm_ffn"])[0], f(inp["g_final"])], 0)),
        "w_q": np.ascontiguousarray(w_in[:, oQ:oQ + DK]), "w_k": np.ascontiguousarray(w_in[:, oK:oK + DK]),
        "w_v": np.ascontiguousarray(w_in[:, oV:oV + DG]), "w_r": np.ascontiguousarray(w_in[:, oR:oR + DG]),
        "w_ab": w_ab, "w_g": np.ascontiguousarray(w_in[:, oGF:oGF + 32]),
        "wgu": np.ascontiguousarray(wgu.reshape(32, DK)), "bgu": bgu,
        "g_gla": f(inp["g_gla_out"])[0], "w_dw": f(inp["w_dw"])[0],
        "cvec": np.ascontiguousarray(np.stack([f(inp["b_dw"])[0], f(inp["g_conv_ln"])[0], f(inp["b_conv_ln"])[0]], 0)),
        "w_out": f(inp["w_out"])[0], "w_rt": f(inp["w_router"])[0], "b_rt": f(inp["b_router"])[0],
        "w_eg": relay(weg), "w_eu": relay(weu), "w_ed": np.ascontiguousarray(f(inp["w_e_down"])[0].reshape(c.E * c.DE, D)),
        "w_sg": f(inp["w_s_gate"])[0], "w_su": f(inp["w_s_up"])[0], "w_sd": f(inp["w_s_down"])[0],
    }
    maps = []
    for i in range(c.NCORES):
        segs = [x[j * TOK:(j + 1) * TOK] for j in range(i)] + [x[j * TOK:(j + 1) * TOK][::-1] for j in range(c.NCORES - 1, i, -1)]
        dirs = [0, 1] + [0] * i + [1] * (c.NCORES - 1 - i)
        m = dict(common)
        m["x_own"] = np.ascontiguousarray(x[i * TOK:(i + 1) * TOK])
        m["x_oth"] = np.ascontiguousarray(np.concatenate(segs, 0)) if segs else np.zeros((TOK, D), np.float32)
        m["w_gs"] = np.ascontiguousarray(np.concatenate([(wgb if d else wgf) for d in dirs], 0))
        m["wgu_s"] = np.ascontiguousarray(np.concatenate([wgu[d] for d in dirs], 0))
        m["bgu_s"] = np.ascontiguousarray(np.stack([bgu[d] for d in dirs], 0))
        m["cm"] = _make_consts(c, i)
        maps.append(m)
    return maps


_CACHE = {}


def kernel(**inputs):
    c = Cfg()
    if "nc" not in _CACHE:
        _CACHE["nc"] = build(c)
    nc = _CACHE["nc"]
    maps = _prep_inputs(c, inputs)
    res = run_bass_kernel_spmd(nc, maps, core_ids=list(range(c.NCORES)))
    outs = [np.asarray(res.results[i]["out"], dtype=np.float32) for i in range(c.NCORES)]
    return np.concatenate(outs, 0).reshape(1, c.SEQ, c.D)
```
